# Optimizing a Trainium2 kernel written in Bass

```python
import jax, jax.numpy as jnp
from jax import lax
import numpy as np

D_MODEL = 1024
BATCH = 32
SEQ = 2048
DEPTH = 1

HEAD_DIM = 64
N_HEADS = D_MODEL // HEAD_DIM
N_HEADS_B = N_HEADS // 4
N_HEADS_A = N_HEADS - N_HEADS_B
WIDTH_A = N_HEADS_A * HEAD_DIM
WIDTH_B = N_HEADS_B * HEAD_DIM
MIX_WIDTH = WIDTH_A + WIDTH_B
DILATED_CONFIGS = ((128, 1), (512, 4), (2048, 16))
MOBA_BLOCK = 256
MOBA_TOPK = 3
D_FF = 2816
LN_EPS = 1e-5

kernel_name = 'hybrid_dilated_moba_macaron_deepnorm'


def layer_norm(x, g, b):
    xf = x.astype(jnp.float32)
    mu = jnp.mean(xf, axis=-1, keepdims=True)
    var = jnp.mean(jnp.square(xf - mu), axis=-1, keepdims=True)
    y = (xf - mu) * lax.rsqrt(var + LN_EPS)
    return (y * g.astype(jnp.float32) + b.astype(jnp.float32)).astype(x.dtype)


def swiglu(h, w_gate, w_up, w_down):
    return (jax.nn.silu(h @ w_gate) * (h @ w_up)) @ w_down


def alibi_slopes():
    idx = np.arange(N_HEADS)
    slopes = 2.0 ** (-8.0 * (idx + 1) / N_HEADS)
    is_b = (idx % 4) == 3
    return (jnp.asarray(slopes[~is_b], dtype=jnp.float32),
            jnp.asarray(slopes[is_b], dtype=jnp.float32))


def dilated_branch(q, k, v, slopes, window, dilation):
    H, S, Dh = q.shape
    d = dilation
    C = window // d
    s_sub = S // d
    nb = -(-s_sub // C)
    L = nb * C

    def to_sub(t):
        return t.reshape(H, s_sub, d, Dh).transpose(0, 2, 1, 3)

    qs, ks, vs = to_sub(q), to_sub(k), to_sub(v)
    qs = jnp.pad(qs, ((0, 0), (0, 0), (0, L - s_sub), (0, 0))).reshape(H, d, nb, C, Dh)

    def band(t):
        tp = jnp.pad(t, ((0, 0), (0, 0), (C, L - s_sub), (0, 0))).reshape(H, d, nb + 1, C, Dh)
        return jnp.concatenate([tp[:, :, :-1], tp[:, :, 1:]], axis=3)

    kw, vw = band(ks), band(vs)
    a = jnp.arange(C)[:, None]
    j = jnp.arange(2 * C)[None, :]
    delta = a + C - j
    key_sub = jnp.arange(nb)[:, None, None] * C - C + j
    valid = (delta >= 0) & (delta <= C) & (key_sub >= 0)

    s = jnp.einsum('hrnqd,hrnkd->hrnqk', qs, kw) * (Dh ** -0.5)
    s = s - slopes[:, None, None, None, None] * (delta * d).astype(jnp.float32)
    s = jnp.where(valid, s, -jnp.inf)
    m = jnp.max(s, axis=-1, keepdims=True)
    p = jnp.exp(s - m)
    l = jnp.sum(p, axis=-1, keepdims=True)
    o = jnp.einsum('hrnqk,hrnkd->hrnqd', p, vw) / l
    lse = (m + jnp.log(l))[..., 0]
    o = o.reshape(H, d, L, Dh)[:, :, :s_sub].transpose(0, 2, 1, 3).reshape(H, S, Dh)
    lse = lse.reshape(H, d, L)[:, :, :s_sub].transpose(0, 2, 1).reshape(H, S)
    return o, lse


def dilated_mixer(q, k, v, slopes):
    outs, lses = [], []
    for window, dilation in DILATED_CONFIGS:
        o, lse = dilated_branch(q, k, v, slopes, window, dilation)
        outs.append(o)
        lses.append(lse)
    wts = jax.nn.softmax(jnp.stack(lses), axis=0)
    return jnp.sum(wts[..., None] * jnp.stack(outs), axis=0)


def moba_mixer(q, k, v, slopes):
    H, S, Dh = q.shape
    Bk = MOBA_BLOCK
    nblk = -(-S // Bk)
    Sp = nblk * Bk
    n_sel = min(MOBA_TOPK, nblk - 1)
    scale = Dh ** -0.5

    def blocks(t):
        return jnp.pad(t, ((0, 0), (0, Sp - S), (0, 0))).reshape(H, nblk, Bk, Dh)

    qb, kb, vb = blocks(q), blocks(k), blocks(v)
    a = jnp.arange(Bk)
    causal = a[:, None] >= a[None, :]
    own_dist = (a[:, None] - a[None, :]).astype(jnp.float32)
    hidx = jnp.arange(H)[:, None, None]

    def own_scores(qn, kn):
        s = jnp.einsum('hqd,hkd->hqk', qn, kn) * scale - slopes[:, None, None] * own_dist
        return jnp.where(causal, s, -jnp.inf)

    if n_sel == 0:
        def step0(args):
            qn, kn, vn = args
            p = jax.nn.softmax(own_scores(qn, kn), axis=-1)
            return jnp.einsum('hqk,hkd->hqd', p, vn)
        o = lax.map(step0, (qb.transpose(1, 0, 2, 3), kb.transpose(1, 0, 2, 3), vb.transpose(1, 0, 2, 3)))
        return o.transpose(1, 0, 2, 3).reshape(H, Sp, Dh)[:, :S]

    kmean = jnp.mean(kb, axis=2)
    gate = jnp.einsum('hnqd,hmd->hnqm', qb, kmean)
    blk_ids = jnp.arange(nblk)
    past = blk_ids[None, :] < blk_ids[:, None]
    gate = jnp.where(past[None, :, None, :], gate, -jnp.inf)
    _, sel = lax.top_k(gate, n_sel)
    sel_ok = sel < blk_ids[None, :, None, None]

    def step(args):
        n, qn, kn, vn, seln, okn = args
        s_own = own_scores(qn, kn)
        kg = kb[hidx, seln]
        vg = vb[hidx, seln]
        dist = (n * Bk + a)[None, :, None, None] - (seln[..., None] * Bk + a[None, None, None, :])
        s_sel = jnp.einsum('hqd,hqskd->hqsk', qn, kg) * scale - slopes[:, None, None, None] * dist.astype(jnp.float32)
        s_sel = jnp.where(okn[..., None], s_sel, -jnp.inf)
        s_all = jnp.concatenate([s_own, s_sel.reshape(H, Bk, n_sel * Bk)], axis=-1)
        p = jax.nn.softmax(s_all, axis=-1)
        p_own = p[..., :Bk]
        p_sel = p[..., Bk:].reshape(H, Bk, n_sel, Bk)
        return jnp.einsum('hqk,hkd->hqd', p_own, vn) + jnp.einsum('hqsk,hqskd->hqd', p_sel, vg)

    xs = (blk_ids, qb.transpose(1, 0, 2, 3), kb.transpose(1, 0, 2, 3), vb.transpose(1, 0, 2, 3),
          sel.transpose(1, 0, 2, 3), sel_ok.transpose(1, 0, 2, 3))
    o = lax.map(step, xs)
    return o.transpose(1, 0, 2, 3).reshape(H, Sp, Dh)[:, :S]


def token_mixer(h, w_in, w_out):
    B, S, _ = h.shape
    proj = h @ w_in
    cuts = [WIDTH_A, 2 * WIDTH_A, 3 * WIDTH_A, 3 * WIDTH_A + WIDTH_B, 3 * WIDTH_A + 2 * WIDTH_B]
    qa, ka, va, qb, kb, vb = jnp.split(proj, cuts, axis=-1)

    def heads(t, nh):
        return t.reshape(B, S, nh, HEAD_DIM).transpose(0, 2, 1, 3).astype(jnp.float32)

    slopes_a, slopes_b = alibi_slopes()
    oa = lax.map(lambda t: dilated_mixer(t[0], t[1], t[2], slopes_a),
                 (heads(qa, N_HEADS_A), heads(ka, N_HEADS_A), heads(va, N_HEADS_A)))
    ob = lax.map(lambda t: moba_mixer(t[0], t[1], t[2], slopes_b),
                 (heads(qb, N_HEADS_B), heads(kb, N_HEADS_B), heads(vb, N_HEADS_B)))
    o = jnp.concatenate([oa.transpose(0, 2, 1, 3).reshape(B, S, WIDTH_A),
                         ob.transpose(0, 2, 1, 3).reshape(B, S, WIDTH_B)], axis=-1)
    return o.astype(h.dtype) @ w_out


def setup_inputs(seed: int = 0) -> dict:
    key = jax.random.key(seed)
    ks = jax.random.split(key, 16)
    beta = (8.0 * DEPTH) ** -0.25

    def nrm(k, shape, scale):
        return jax.random.normal(k, shape, jnp.float32) * scale

    col_scale = jnp.concatenate([
        jnp.ones((2 * WIDTH_A,), jnp.float32), jnp.full((WIDTH_A,), beta, jnp.float32),
        jnp.ones((2 * WIDTH_B,), jnp.float32), jnp.full((WIDTH_B,), beta, jnp.float32)])
    return {
        'x': nrm(ks[0], (BATCH, SEQ, D_MODEL), 1.0),
        'ffn1_gate': nrm(ks[1], (DEPTH, D_MODEL, D_FF), D_MODEL ** -0.5),
        'ffn1_up': nrm(ks[2], (DEPTH, D_MODEL, D_FF), D_MODEL ** -0.5),
        'ffn1_down': nrm(ks[3], (DEPTH, D_FF, D_MODEL), beta * D_FF ** -0.5),
        'ln1_g': 1.0 + nrm(ks[4], (DEPTH, D_MODEL), 0.05),
        'ln1_b': nrm(ks[5], (DEPTH, D_MODEL), 0.02),
        'w_in': nrm(ks[6], (DEPTH, D_MODEL, 3 * MIX_WIDTH), D_MODEL ** -0.5) * col_scale,
        'w_out': nrm(ks[7], (DEPTH, MIX_WIDTH, D_MODEL), beta * MIX_WIDTH ** -0.5),
        'ln2_g': 1.0 + nrm(ks[8], (DEPTH, D_MODEL), 0.05),
        'ln2_b': nrm(ks[9], (DEPTH, D_MODEL), 0.02),
        'ffn2_gate': nrm(ks[10], (DEPTH, D_MODEL, D_FF), D_MODEL ** -0.5),
        'ffn2_up': nrm(ks[11], (DEPTH, D_MODEL, D_FF), D_MODEL ** -0.5),
        'ffn2_down': nrm(ks[12], (DEPTH, D_FF, D_MODEL), beta * D_FF ** -0.5),
        'ln3_g': 1.0 + nrm(ks[13], (DEPTH, D_MODEL), 0.05),
        'ln3_b': nrm(ks[14], (DEPTH, D_MODEL), 0.02),
    }


def reference(x, ffn1_gate, ffn1_up, ffn1_down, ln1_g, ln1_b, w_in, w_out, ln2_g, ln2_b,
              ffn2_gate, ffn2_up, ffn2_down, ln3_g, ln3_b):
    alpha = (2.0 * DEPTH) ** 0.25
    h = x
    for l in range(DEPTH):
        h = layer_norm(alpha * h + 0.5 * swiglu(h, ffn1_gate[l], ffn1_up[l], ffn1_down[l]), ln1_g[l], ln1_b[l])
        h = layer_norm(alpha * h + token_mixer(h, w_in[l], w_out[l]), ln2_g[l], ln2_b[l])
        h = layer_norm(alpha * h + 0.5 * swiglu(h, ffn2_gate[l], ffn2_up[l], ffn2_down[l]), ln3_g[l], ln3_b[l])
    return h
```

```python
import math
from contextlib import ExitStack

import numpy as np
import ml_dtypes

import concourse.bass as bass
import concourse.mybir as mybir
from concourse.bass_utils import run_bass_kernel_spmd

F32 = mybir.dt.float32
BF16 = mybir.dt.bfloat16
AF = mybir.ActivationFunctionType
ALU = mybir.AluOpType

NCORES = 8


class Buf:
    __slots__ = ("name", "psum", "writers", "readers")

    def __init__(self, name, psum=False):
        self.name = name
        self.psum = psum
        self.writers = []
        self.readers = []


class Op:
    __slots__ = ("eng", "fn", "deps", "is_dma", "sem", "val", "marked", "idx", "prev_lane")

    def __init__(self, eng, fn, is_dma=False):
        self.eng = eng
        self.fn = fn
        self.deps = []
        self.is_dma = is_dma
        self.sem = None
        self.val = None
        self.marked = False
        self.idx = None
        self.prev_lane = None


ENGINES = ("pe", "act", "dve", "pool", "sp")


class Sched:
    def __init__(self, nc, es, lanes_sp=8, lanes_pool=6):
        self.nc = nc
        self.ops = {e: [] for e in ENGINES}
        self.esem = {e: es.enter_context(nc.semaphore("sem_" + e)) for e in ENGINES}
        self.lanes = {
            "sp": [es.enter_context(nc.semaphore(f"dsp{i}")) for i in range(lanes_sp)],
            "pool": [es.enter_context(nc.semaphore(f"dpl{i}")) for i in range(lanes_pool)],
        }
        self.lane_rr = {"sp": 0, "pool": 0}
        self.lane_val = {}
        self.lane_last = {}

    @staticmethod
    def _merge(lst, op):
        if not op.is_dma:
            lst[:] = [o for o in lst if o.is_dma or o.eng != op.eng]
        lst.append(op)

    def _track(self, op, reads, writes):
        deps = op.deps
        for b in reads:
            for w in b.writers:
                deps.append(w)
            if b.psum:
                for r in b.readers:
                    if r.eng != op.eng:
                        deps.append(r)
        for b in writes:
            for w in b.writers:
                if w.is_dma or op.is_dma or w.eng != op.eng:
                    deps.append(w)
            for r in b.readers:
                if r.is_dma or op.is_dma or r.eng != op.eng:
                    deps.append(r)
        for b in reads:
            self._merge(b.readers, op)
        for b in writes:
            b.writers = [op]
            b.readers = []
        for d in deps:
            d.marked = True

    def op(self, eng, fn, reads=(), writes=()):
        o = Op(eng, fn)
        self._track(o, reads, writes)
        self.ops[eng].append(o)
        return o

    def dma(self, q, out, in_, reads=(), writes=()):
        o = Op(q, lambda e: e.dma_start(out=out, in_=in_), is_dma=True)
        lanes = self.lanes[q]
        li = self.lane_rr[q]
        self.lane_rr[q] = (li + 1) % len(lanes)
        sem = lanes[li]
        o.sem = sem
        o.val = self.lane_val.get(id(sem), 0) + 16
        self.lane_val[id(sem)] = o.val
        o.prev_lane = self.lane_last.get(id(sem))
        self.lane_last[id(sem)] = o
        self._track(o, reads, writes)
        self.ops[q].append(o)
        return o

    def emit(self, block):
        nc = self.nc
        for e in ENGINES:
            c = 0
            for o in self.ops[e]:
                if not o.is_dma:
                    if o.marked:
                        c += 1
                        o.sem = self.esem[e]
                        o.val = c
        final_waits = []
        for q in ("sp", "pool"):
            for sem in self.lanes[q]:
                last = self.lane_last.get(id(sem))
                if last is not None:
                    final_waits.append((sem, last.val))

        def run(e, eng):
            waited = {}
            for o in self.ops[e]:
                need = {}
                deps = list(o.deps)
                if o.is_dma and o.prev_lane is not None:
                    deps.append(o.prev_lane)
                for d in deps:
                    k = id(d.sem)
                    if waited.get(k, 0) >= d.val:
                        continue
                    if k not in need or need[k][1] < d.val:
                        need[k] = (d.sem, d.val)
                for k, (sem, val) in need.items():
                    eng.wait_ge(sem, val)
                    waited[k] = val
                inst = o.fn(eng)
                if o.is_dma:
                    inst.then_inc(o.sem, 16)
                elif o.marked:
                    inst.then_inc(o.sem, 1)
            if e == "sp":
                for sem, val in final_waits:
                    eng.wait_ge(sem, val)

        @block.tensor
        def _(eng):
            run("pe", eng)

        @block.scalar
        def _(eng):
            run("act", eng)

        @block.vector
        def _(eng):
            run("dve", eng)

        @block.gpsimd
        def _(eng):
            run("pool", eng)

        @block.sync
        def _(eng):
            run("sp", eng)


SEQ = 2048
DM = 1024
DFF = 2816
NT = SEQ // 128
KC = DM // 128
FC = DFF // 128
BATCH = 32
BPC = BATCH // NCORES
ALPHA = 2.0 ** 0.25
EPS_LN = 1e-5 / (ALPHA * ALPHA)
C_FFN = 0.5 / ALPHA
C_MIX = 1.0 / ALPHA
FGROUPS = [(0, 6), (6, 12), (12, 17), (17, 22)]
BIG = 1.0e9
NEGB = -30000.0
NORM_MODE = 0

_idx = np.arange(16)
_sl = 2.0 ** (-8.0 * (_idx + 1) / 16)
SLOPES_A = [float(v) for v in _sl[(_idx % 4) != 3]]
SLOPES_B = [float(v) for v in _sl[(_idx % 4) == 3]]


def _const_tables():
    k = np.arange(128)[:, None].astype(np.float64)
    q = np.arange(128)[None, :].astype(np.float64)
    own = np.where(q >= k, q - k, BIG)
    prev = np.where(q <= k, 128 + q - k, BIG)
    d1 = np.concatenate([prev, own, prev, own], axis=1)
    d3 = []
    for j in range(4):
        c = (32 * j + np.arange(32))[None, :].astype(np.float64)
        u = np.where(c >= k, c - k, BIG)
        d3.append(np.tile(u, (1, 16)))
    q2 = np.arange(256)[None, :].astype(np.float64)
    dmp = np.concatenate([q2 - k, q2 - 128 - k], axis=1)
    dmo = np.where(dmp >= 0, 0.0, BIG)
    dtab = np.concatenate([d1] + d3 + [dmp, dmo], axis=1)
    neg = np.zeros((128, 16, 8), np.float32)
    ownb = np.zeros((128, 16, 8), np.float32)
    for t in range(16):
        n = t // 2
        neg[:, t, n:] = -1.0e30
        ownb[:, t, n] = 1.0
    gcon = np.concatenate([neg.reshape(128, 128), ownb.reshape(128, 128)], axis=1)
    loc = (np.arange(SEQ) % 256).astype(np.float32)
    erows = np.zeros((4, 12, SEQ), np.float32)
    for hb in range(4):
        for m in range(8):
            erows[hb, m, 256 * m:256 * (m + 1)] = 1.0
        erows[hb, 8] = 1.0
        erows[hb, 9] = SLOPES_B[hb] * loc
        erows[hb, 10] = -SLOPES_B[hb] * loc
        erows[hb, 11] = 1.0
    erows = erows.reshape(48, SEQ)
    return (dtab.astype(ml_dtypes.bfloat16), gcon.astype(np.float32),
            erows.astype(ml_dtypes.bfloat16), np.eye(128).astype(ml_dtypes.bfloat16))


def build_program(n_seq=BPC, dbg=False):
    nc = bass.Bass("TRN2", target_bir_lowering=False)

    def dram(name, shape, dtype, kind):
        return nc.dram_tensor(name, shape, dtype, kind=kind).ap()

    x = dram("x", [n_seq * SEQ, DM], F32, "ExternalInput")
    out = dram("out", [n_seq * SEQ, DM], F32, "ExternalOutput")
    wgu_f = [dram(f"wgu{i}", [FC * 128, 2048], F32, "ExternalInput") for i in (1, 2)]
    wd_f = [dram(f"wd{i}", [128, FC * 1024], F32, "ExternalInput") for i in (1, 2)]
    win_f = dram("win", [8 * 128, 3072], F32, "ExternalInput")
    wout_f = dram("wout", [8 * 128, 1024], F32, "ExternalInput")
    ln_d = dram("ln", [6, DM], F32, "ExternalInput")
    dtab_d = dram("dtab", [128, 3584], BF16, "ExternalInput")
    gcon_d = dram("gcon", [128, 256], F32, "ExternalInput")
    erows_d = dram("erows", [48, SEQ], BF16, "ExternalInput")
    ident_d = dram("ident", [128, 128], BF16, "ExternalInput")
    wgu_b = [dram(f"wgu{i}b", [FC * 128, 2048], BF16, "Internal") for i in (1, 2)]
    wd_b = [dram(f"wd{i}b", [128, FC * 1024], BF16, "Internal") for i in (1, 2)]
    win_b = dram("winb", [8 * 128, 3072], BF16, "Internal")
    wout_b = dram("woutb", [8 * 128, 1024], BF16, "Internal")
    if dbg:
        dbg_h1 = dram("dbg_h1", [SEQ, DM], F32, "ExternalOutput")
        dbg_h2 = dram("dbg_h2", [SEQ, DM], F32, "ExternalOutput")

    es = ExitStack()
    with es:
        S = Sched(nc, es, lanes_sp=10, lanes_pool=30)

        def sb(name, shape, dtype):
            return es.enter_context(nc.sbuf_tensor(name, shape, dtype))

        R = sb("R", [128, NT, DM], F32)
        hT = sb("hT", [128, KC, SEQ], BF16)
        lnp_s = [sb(f"lnp{i}", [128, 2, DM], F32) for i in range(2)]
        ident = sb("ident_sb", [128, 128], BF16)
        dtab = sb("dtab_sb", [128, 3584], BF16)
        gcon = sb("gcon_sb", [128, 256], F32)
        mones = sb("mones", [64, 512], F32)
        stt_s = [sb(f"stt{i}", [128, 4, 2, 6], F32) for i in range(2)]
        mv_s = [sb(f"mv{i}", [128, 4, 2], F32) for i in range(2)]
        rs_s = [sb(f"rs{i}", [128, 3, 4], F32) for i in range(2)]
        nbias_s = [sb(f"nbias{i}", [128, 4], F32) for i in range(2)]
        UEL = 40960
        U = sb("U", [128, UEL], BF16)
        ps = es.enter_context(nc.psum_tensor("ps", [128, 4096], F32))
        block = es.enter_context(nc.Block())

        def bank(i, n=1):
            return ps[:, 512 * i:512 * (i + n)]

        PB = [Buf(f"bank{i}", psum=True) for i in range(8)]

        arena = []

        class ABuf(Buf):
            __slots__ = ("lo", "hi", "over")

            def __init__(self, name, lo, hi):
                super().__init__(name)
                self.lo, self.hi = lo, hi
                self.over = []
                for o in arena:
                    if o.lo < hi and lo < o.hi:
                        o.over.append(self)
                        self.over.append(o)
                arena.append(self)

        _orig_track = S._track

        def _track(op, reads, writes):
            for b in writes:
                if isinstance(b, ABuf):
                    for ob in b.over:
                        for w in ob.writers + ob.readers:
                            if w.is_dma or op.is_dma or w.eng != op.eng:
                                op.deps.append(w)
            for b in reads:
                if isinstance(b, ABuf):
                    for ob in b.over:
                        for w in ob.writers:
                            op.deps.append(w)
            _orig_track(op, reads, writes)

        S._track = _track

        def uview(lo, n, dtype=BF16, parts=128):
            ap = U[0:parts, lo:lo + n]
            if dtype == F32:
                ap = ap.bitcast(F32)
            return ap

        wd_sl = [uview(6144 * i, 6144).rearrange("p (f d) -> p f d", d=1024) for i in range(2)]
        wd_bf = [ABuf(f"wd_sl{i}", 6144 * i, 6144 * (i + 1)) for i in range(2)]
        act_sl = [uview(12288 + 3072 * i, 3072).rearrange("p (f t) -> p f t", t=512) for i in range(2)]
        act_bf = [ABuf(f"act{i}", 12288 + 3072 * i, 12288 + 3072 * (i + 1)) for i in range(2)]
        wgu_sl = [uview(18432 + 2048 * i, 2048).rearrange("p (g k f) -> p g k f", g=2, k=8) for i in range(3)]
        wgu_bf = [ABuf(f"wgu{i}", 18432 + 2048 * i, 18432 + 2048 * (i + 1)) for i in range(3)]
        hTs = uview(24576, 4096).rearrange("p (k t) -> p k t", t=512)
        hTs_bf = ABuf("hTs", 24576, 28672)
        _xo = [28672, 29696, 32768, 33792]
        xb = [uview(o, 1024) for o in _xo]
        xb_bf = [ABuf(f"xb{i}", o, o + 1024) for i, o in enumerate(_xo)]
        sg = [uview(30720 + 1024 * i, 1024, F32) for i in range(2)]
        sg_bf = [ABuf(f"sg{i}", 30720 + 1024 * i, 30720 + 1024 * (i + 1)) for i in range(2)]
        win_sl = [uview(3072 * i, 3072).rearrange("p (t k c) -> p t k c", t=3, k=8) for i in range(2)]
        win_bf = [ABuf(f"win{i}", 3072 * i, 3072 * (i + 1)) for i in range(2)]
        wout_sl = [uview(6144 + 1024 * i, 1024) for i in range(2)]
        wout_bf = [ABuf(f"wout{i}", 6144 + 1024 * i, 6144 + 1024 * (i + 1)) for i in range(2)]
        qb = uview(8192, 2048)
        kb = uview(10240, 2048)
        qb_bf = ABuf("qb", 8192, 10240)
        kb_bf = ABuf("kb", 10240, 12288)
        qk_sets = [(qb, kb, qb_bf, kb_bf),
                   (uview(36864, 2048), uview(38912, 2048), ABuf("qb2", 36864, 38912), ABuf("kb2", 38912, 40960))]
        vaug = [uview(12288 + 4096 * i, 4096).rearrange("p (a h c) -> p a h c", h=2, c=128) for i in range(3)]
        vaug_bf = [ABuf(f"vaug{i}", 12288 + 4096 * i, 12288 + 4096 * (i + 1)) for i in range(3)]
        oT = [uview(24576 + 2048 * i, 2048) for i in range(2)]
        oT_bf = [ABuf(f"oT{i}", 24576 + 2048 * i, 24576 + 2048 * (i + 1)) for i in range(2)]
        pT = [uview(28672 + 512 * i, 512) for i in range(4)]
        pT_bf = [ABuf(f"pT{i}", 28672 + 512 * i, 28672 + 512 * (i + 1)) for i in range(4)]
        sbias = [uview(30720 + 1024 * i, 1024, F32) for i in range(3)]
        sbias_bf = [ABuf(f"sbias{i}", 30720 + 1024 * i, 30720 + 1024 * (i + 1)) for i in range(3)]
        rl = [uview(33792 + 1024 * i, 1024, F32, parts=64) for i in range(2)]
        rlT = [uview(33792 + 1024 * i, 512, F32) for i in range(2)]
        rl2 = [uview(33792 + 1024 * i + 512, 512, F32, parts=64) for i in range(2)]
        rl_bf = [ABuf(f"rl{i}", 33792 + 1024 * i, 33792 + 1024 * (i + 1)) for i in range(2)]
        gsb = uview(35840, 256, F32).rearrange("p (a m) -> p a m", m=8)
        top8 = uview(36096, 256, F32).rearrange("p (a m) -> p a m", m=8)
        thr = uview(36352, 32, F32)
        ind = uview(36384, 256, F32).rearrange("p (a m) -> p a m", m=8)
        bsel = uview(36640, 128).rearrange("p (a m) -> p a m", m=8)
        kmf = uview(36768, 16, F32, parts=64)
        kmh = uview(36784, 8, parts=64)
        kml = uview(36792, 8, parts=64)
        kmd = uview(36800, 16, F32, parts=64)
        misc_bf = ABuf("moba_misc", 35840, 36864)

        R_bf = [Buf(f"R{t}") for t in range(NT)]
        hT_bf = [Buf(f"hT{t}") for t in range(NT)]
        lnp_bfs = [Buf("lnp0"), Buf("lnp1")]
        ln_state = {"cur": 0}

        def load_ln(ln_idx):
            b = (ln_state["cur"] + 1) % 2
            ln_state["cur"] = b
            bg_require([("lnp", b)])
            S.dma("pool", lnp_s[b][:], ln_d[2 * ln_idx:2 * ln_idx + 2, :].partition_broadcast(128), writes=[lnp_bfs[b]])
        const_bf = Buf("const")
        st_bfs = [Buf("stats0"), Buf("stats1")]
        import collections as _c
        bg = _c.deque()

        def bg_pop(k=1):
            for _ in range(k):
                if bg:
                    bg.popleft()[1]()

        def bg_flush():
            while bg:
                bg.popleft()[1]()

        def bg_require(tiles):
            tiles = set(tiles)
            last = -1
            for i, (tg, _) in enumerate(bg):
                if tg & tiles:
                    last = i
            for _ in range(last + 1):
                bg.popleft()[1]()
        out_bf = Buf("out_dram")
        wgub_bf = [[Buf(f"wgub{i}_{f}") for f in range(FC)] for i in range(2)]
        wdb_bf = [[Buf(f"wdb{i}_{g}") for g in range(4)] for i in range(2)]
        winb_bf = [Buf(f"winb{p}") for p in range(8)]
        woutb_bf = [Buf(f"woutb{p}") for p in range(8)]

        rot = {"gu": 0, "wgu": 0, "y": 0, "sg": 0, "xb": 0, "proj": 0, "st": 0, "pt": 0, "sb": 0,
               "o": 0, "rl": 0, "ybo": 0, "lnset": 0}

        def nxt(k, n):
            v = rot[k]
            rot[k] = (v + 1) % n
            return v

        S.dma("sp", ident[:], ident_d, writes=[const_bf])
        S.dma("sp", dtab[:], dtab_d, writes=[const_bf])
        S.dma("sp", gcon[:], gcon_d, writes=[const_bf])
        S.op("dve", lambda e: e.memset(mones[:], -1.0), writes=[const_bf])

        def cast_ffn(i):
            for f in range(FC):
                S.dma("pool", wgu_b[i][128 * f:128 * (f + 1), :], wgu_f[i][128 * f:128 * (f + 1), :],
                      writes=[wgub_bf[i][f]])
                if f in (5, 11, 16, 21):
                    g = (5, 11, 16, 21).index(f)
                    f0, f1 = FGROUPS[g]
                    S.dma("pool", wd_b[i][:, 1024 * f0:1024 * f1], wd_f[i][:, 1024 * f0:1024 * f1],
                          writes=[wdb_bf[i][g]])

        def cast_attn():
            for p in range(8):
                S.dma("pool", win_b[128 * p:128 * (p + 1), :], win_f[128 * p:128 * (p + 1), :], writes=[winb_bf[p]])
                S.dma("pool", wout_b[128 * p:128 * (p + 1), :], wout_f[128 * p:128 * (p + 1), :], writes=[woutb_bf[p]])

        def ln_tasks(tiles, post):
            n = len(tiles)
            si_ = nxt("lnset", 2)
            stt, mv, rs, nbias, st_bf = stt_s[si_], mv_s[si_], rs_s[si_], nbias_s[si_], st_bfs[si_]
            lb = ln_state["cur"]
            lnp, lnp_bf = lnp_s[lb], lnp_bfs[lb]
            tasks = []

            def stats(i, t):
                for h in range(2):
                    S.op("dve", lambda e, h=h: e.bn_stats(out=stt[:, i, h, :], in_=R[:, t, 512 * h:512 * (h + 1)]),
                         reads=[R_bf[t]], writes=[st_bf])
                S.op("dve", lambda e: e.bn_aggr(out=mv[:, i, :], in_=stt[:, i, :, :]), reads=[st_bf], writes=[st_bf])

            def chain():
                S.op("dve", lambda e: e.tensor_scalar(out=rs[:, 0, 0:n], in0=mv[:, 0:n, 1], scalar1=EPS_LN, scalar2=None, op0=ALU.add),
                     reads=[st_bf], writes=[st_bf])
                S.op("act", lambda e: e.activation(out=rs[:, 1, 0:n], in_=rs[:, 0, 0:n], func=AF.Sqrt), reads=[st_bf], writes=[st_bf])
                S.op("dve", lambda e: e.reciprocal(out=rs[:, 2, 0:n], in_=rs[:, 1, 0:n]), reads=[st_bf], writes=[st_bf])
                S.op("dve", lambda e: e.scalar_tensor_tensor(out=nbias[:, 0:n], in0=mv[:, 0:n, 0], scalar=-1.0, in1=rs[:, 2, 0:n],
                                                             op0=ALU.mult, op1=ALU.mult), reads=[st_bf], writes=[st_bf])

            def norm(i, t):
                S.op("act", lambda e: e.activation(out=R[:, t, :], in_=R[:, t, :], func=AF.Identity,
                                                   scale=rs[:, 2, i:i + 1], bias=nbias[:, i:i + 1]),
                     reads=[R_bf[t], st_bf], writes=[R_bf[t]])
                S.op("dve", lambda e: e.tensor_tensor(out=R[:, t, :], in0=R[:, t, :], in1=lnp[:, 0, :], op=ALU.mult),
                     reads=[R_bf[t], lnp_bf], writes=[R_bf[t]])
                S.op("dve", lambda e: e.tensor_tensor(out=R[:, t, :], in0=R[:, t, :], in1=lnp[:, 1, :], op=ALU.add),
                     reads=[R_bf[t], lnp_bf], writes=[R_bf[t]])

            for i, t in enumerate(tiles):
                tasks.append(({t}, lambda i=i, t=t: stats(i, t)))
            tasks.append((set(tiles), chain))
            slots = {}
            for i, t in enumerate(tiles):
                slots.setdefault(2 * i, []).append(({t, ("lnp", lb)}, lambda i=i, t=t: norm(i, t)))
                for k, fn in enumerate(post(t)):
                    slots.setdefault(2 * i + 3 * (k + 1), []).append(({t}, fn))
            for k in sorted(slots):
                tasks.extend(slots[k])
            return tasks

        def cast_bf16(t):
            xi = nxt("xb", 4)
            S.op("act", lambda e: e.activation(out=xb[xi], in_=R[:, t, :], func=AF.Copy), reads=[R_bf[t]], writes=[xb_bf[xi]])
            return xi

        def transpose_to(xi, dst, dst_bf, col0):
            psb = bank(7).bitcast(BF16)
            for kc in range(KC):
                S.op("pe", lambda e, kc=kc: e.transpose(out=psb[:, 128 * kc:128 * (kc + 1)], in_=xb[xi][:, 128 * kc:128 * (kc + 1)],
                                                         identity=ident[:]),
                     reads=[xb_bf[xi], const_bf], writes=[PB[7]])
            S.op("act", lambda e: e.activation(out=dst[:, :, col0:col0 + 128],
                                               in_=psb.rearrange("p (k t) -> p k t", t=128), func=AF.Copy),
                 reads=[PB[7]], writes=[dst_bf])

        def ffn_phase(s, wi, ln_idx, first):
            load_ln(ln_idx)

            def load_x(sbk):
                bg_require(range(4 * sbk, 4 * sbk + 4))
                for tt in range(4):
                    t = 4 * sbk + tt
                    r0 = s * SEQ + 128 * t
                    S.dma("sp", R[:, t, :], x[r0:r0 + 128, :], writes=[R_bf[t]])

            prep_slots = {}

            def prep_cast(sbk):
                bg_require(range(4 * sbk, 4 * sbk + 4))
                prep_slots[sbk] = [cast_bf16(4 * sbk + tt) for tt in range(4)]

            def prep(sbk):
                for tt in range(4):
                    transpose_to(prep_slots[sbk][tt], hTs, hTs_bf, 128 * tt)

            def load_wd(g):
                f0, f1 = FGROUPS[g]
                S.dma("sp", wd_sl[g % 2][:, 0:f1 - f0, :],
                      wd_b[wi][:, 1024 * f0:1024 * f1].rearrange("p (f d) -> p f d", d=1024),
                      reads=[wdb_bf[wi][g]], writes=[wd_bf[g % 2]])

            def gate_up(g):
                f0, f1 = FGROUPS[g]
                for f in range(f0, f1):
                    ws = nxt("wgu", 3)
                    S.dma("sp", wgu_sl[ws], wgu_b[wi][128 * f:128 * (f + 1), :].rearrange("p (g k f) -> p g k f", g=2, k=8),
                          reads=[wgub_bf[wi][f]], writes=[wgu_bf[ws]])
                    banks = []
                    for gu in range(2):
                        b = nxt("gu", 3)
                        banks.append(b)
                        for kc in range(KC):
                            S.op("pe", lambda e, b=b, gu=gu, kc=kc, ws=ws: e.matmul(
                                bank(b), lhsT=wgu_sl[ws][:, gu, kc, :], rhs=hTs[:, kc, :], start=(kc == 0), stop=(kc == KC - 1)),
                                reads=[wgu_bf[ws], hTs_bf], writes=[PB[b]])
                    si = nxt("sg", 2)
                    S.op("act", lambda e, b=banks[0], si=si: e.activation(out=sg[si], in_=bank(b), func=AF.Silu),
                         reads=[PB[banks[0]]], writes=[sg_bf[si]])
                    S.op("dve", lambda e, b=banks[1], si=si, g=g, fl=f - f0: e.tensor_tensor(
                        out=act_sl[g % 2][:, fl, :], in0=sg[si], in1=bank(b), op=ALU.mult),
                        reads=[sg_bf[si], PB[banks[1]]], writes=[act_bf[g % 2]])
                    bg_pop(2)

            def down(sbk, g):
                f0, f1 = FGROUPS[g]
                nf = f1 - f0
                for tt in range(4):
                    t = 4 * sbk + tt
                    yb = 3 + 2 * nxt("y", 2)
                    for fl in range(nf):
                        for h in range(2):
                            S.op("pe", lambda e, fl=fl, h=h, yb=yb, tt=tt: e.matmul(
                                bank(yb + h), lhsT=act_sl[g % 2][:, fl, 128 * tt:128 * (tt + 1)],
                                rhs=wd_sl[g % 2][:, fl, 512 * h:512 * (h + 1)], start=(fl == 0), stop=(fl == nf - 1)),
                                reads=[act_bf[g % 2], wd_bf[g % 2]], writes=[PB[yb + h]])
                    S.op("dve", lambda e, yb=yb, t=t: e.scalar_tensor_tensor(
                        out=R[:, t, :], in0=bank(yb, 2), scalar=C_FFN, in1=R[:, t, :], op0=ALU.mult, op1=ALU.add),
                        reads=[PB[yb], PB[yb + 1], R_bf[t]], writes=[R_bf[t]])

            def post1(t):
                st = {}

                def a():
                    st["xi"] = cast_bf16(t)
                    if dbg:
                        S.dma("pool", dbg_h1[128 * t:128 * (t + 1), :], R[:, t, :], reads=[R_bf[t]], writes=[out_bf])

                def b():
                    transpose_to(st["xi"], hT, hT_bf[t], 128 * t)
                return [a, b]

            def post2(t):
                def a():
                    r0 = s * SEQ + 128 * t
                    S.dma("pool", out[r0:r0 + 128, :], R[:, t, :], reads=[R_bf[t]], writes=[out_bf])
                return [a]

            if first:
                load_x(0)
            prep_cast(0)
            prep(0)
            for sbk in range(4):
                if first and sbk + 1 < 4:
                    load_x(sbk + 1)
                for g in range(4):
                    load_wd(g)
                    if g == 3 and sbk + 1 < 4:
                        prep_cast(sbk + 1)
                    gate_up(g)
                    if g == 3 and sbk + 1 < 4:
                        prep(sbk + 1)
                    down(sbk, g)
                bg.extend(ln_tasks([4 * sbk + tt for tt in range(4)], post1 if first else post2))

        def attn_phase(s):
            load_ln(1)
            for o in range(3):
                for hh in range(2):
                    S.op("dve", lambda e, o=o, hh=hh: e.memset(vaug[o][:, :, hh, 64:128], 1.0), writes=[vaug_bf[o]])

            def load_w(pi):
                S.dma("sp", win_sl[pi % 2], win_b[128 * pi:128 * (pi + 1), :].rearrange("p (t k c) -> p t k c", t=3, k=8),
                      reads=[winb_bf[pi]], writes=[win_bf[pi % 2]])

            def load_wout(pi):
                S.dma("sp", wout_sl[pi % 2], wout_b[128 * pi:128 * (pi + 1), :], reads=[woutb_bf[pi]], writes=[wout_bf[pi % 2]])

            hT_all = list(hT_bf)

            def proj_qk(pi, t, dst, dst_bf, rows=None, chunks=(0, 1, 2, 3), pops=0, defer=None):
                w = win_sl[pi % 2]

                def one(c):
                    b = nxt("proj", 2)
                    for kc in range(KC):
                        if rows is None:
                            lhsT = w[:, t, kc, :]
                            o_ap = bank(b)
                        else:
                            lhsT = w[:, t, kc, rows[0]:rows[1]]
                            o_ap = bank(b)[0:64, :]
                        S.op("pe", lambda e, lhsT=lhsT, o_ap=o_ap, kc=kc, c=c: e.matmul(
                            o_ap, lhsT=lhsT, rhs=hT[:, kc, 512 * c:512 * (c + 1)], start=(kc == 0), stop=(kc == KC - 1)),
                            reads=[win_bf[pi % 2]] + hT_all[4 * c:4 * c + 4], writes=[PB[b]])
                    np_ = 128 if rows is None else 64
                    S.op("act", lambda e, b=b, c=c, np_=np_: e.activation(
                        out=dst[0:np_, 512 * c:512 * (c + 1)], in_=bank(b)[0:np_, :], func=AF.Copy,
                        scale=(0.125 if t == 0 else 1.0)),
                        reads=[PB[b]], writes=[dst_bf])

                for c in chunks:
                    if defer is not None:
                        defer.append(lambda c=c: one(c))
                    else:
                        bg_pop(pops)
                        one(c)

            def tok_ap(o, tile, kc):
                if o == 0:
                    return hT[:, kc, 128 * tile:128 * (tile + 1)]
                if o == 1:
                    r4, j = tile // 4, tile % 4
                    return hT[:, kc, 512 * j + r4:512 * (j + 1):4]
                return hT[:, kc, tile:SEQ:16]

            def proj_v(pi, orders):
                w = win_sl[pi % 2]
                for o in orders:
                    for grp in range(4):
                        b = nxt("proj", 2)
                        for ti in range(4):
                            tile = 4 * grp + ti
                            for kc in range(KC):
                                S.op("pe", lambda e, b=b, ti=ti, tile=tile, kc=kc, o=o: e.matmul(
                                    bank(b)[:, 128 * ti:128 * (ti + 1)], lhsT=tok_ap(o, tile, kc), rhs=w[:, 2, kc, :],
                                    start=(kc == 0), stop=(kc == KC - 1), skip_group_check=True),
                                    reads=[win_bf[pi % 2]] + hT_all, writes=[PB[b]])
                        S.op("act", lambda e, b=b, grp=grp, o=o: e.activation(
                            out=vaug[o][:, 4 * grp:4 * grp + 4, :, 0:64],
                            in_=bank(b).rearrange("p (a h c) -> p a h c", h=2, c=64), func=AF.Copy),
                            reads=[PB[b]], writes=[vaug_bf[o]])

            pend = []
            LAG = 3
            ST_BANKS = (2, 3, 4, 7)
            fillers = []
            ucount = {"n": 0}

            def filler_pop(force=False):
                ucount["n"] += 1
                if fillers and (force or ucount["n"] % 3 == 0):
                    fillers.pop(0)()

            def filler_flush():
                while fillers:
                    fillers.pop(0)()

            def flush(k):
                while len(pend) > k:
                    pend.pop(0)()

            def unit(st_fn, dt_ap, col_view, scale, exp_bias, pv_fn, kq_bufs):
                b = ST_BANKS[nxt("st", 4)]
                st_fn(bank(b), PB[b], kq_bufs)
                pi_ = nxt("pt", 4)
                if dt_ap is None:
                    S.op("act", lambda e: e.activation(out=col_view(pT[pi_]), in_=col_view(bank(b)), func=AF.Exp, bias=exp_bias),
                         reads=[PB[b]], writes=[pT_bf[pi_]])
                else:
                    si = nxt("sb", 3)
                    S.op("dve", lambda e: e.scalar_tensor_tensor(
                        out=col_view(sbias[si]), in0=col_view(dt_ap), scalar=scale, in1=col_view(bank(b)), op0=ALU.mult, op1=ALU.add),
                        reads=[PB[b], const_bf], writes=[sbias_bf[si]])
                    S.op("act", lambda e: e.activation(out=col_view(pT[pi_]), in_=col_view(sbias[si]), func=AF.Exp, bias=exp_bias),
                         reads=[sbias_bf[si]], writes=[pT_bf[pi_]])
                pend.append(lambda: pv_fn(pT[pi_], pT_bf[pi_]))
                flush(LAG)
                filler_pop()

            def normalize(ob, ncols, hh, obuf, col0):
                ri = nxt("rl", 2)
                if NORM_MODE == 4:
                    h2 = ncols // 2
                    T, T2 = rlT[ri], rl2[ri]
                    S.op("act", lambda e: e.activation(out=T[0:64, 0:h2], in_=bank(ob)[64:128, 0:h2], func=AF.Copy),
                         reads=[PB[ob]], writes=[rl_bf[ri]])
                    S.op("act", lambda e: e.activation(out=T[64:128, 0:h2], in_=bank(ob)[64:128, h2:ncols], func=AF.Copy),
                         reads=[PB[ob]], writes=[rl_bf[ri]])
                    S.op("dve", lambda e: e.reciprocal(out=T[:, 0:h2], in_=T[:, 0:h2]), reads=[rl_bf[ri]], writes=[rl_bf[ri]])
                    S.op("act", lambda e: e.activation(out=T2[:, 0:h2], in_=T[64:128, 0:h2], func=AF.Copy),
                         reads=[rl_bf[ri]], writes=[rl_bf[ri]])
                    r_ = slice(64 * hh, 64 * hh + 64)
                    S.op("dve", lambda e: e.tensor_tensor(out=oT[obuf][r_, col0:col0 + h2], in0=bank(ob)[0:64, 0:h2],
                                                          in1=T[0:64, 0:h2], op=ALU.mult),
                         reads=[PB[ob], rl_bf[ri]], writes=[oT_bf[obuf]])
                    S.op("dve", lambda e: e.tensor_tensor(out=oT[obuf][r_, col0 + h2:col0 + ncols], in0=bank(ob)[0:64, h2:ncols],
                                                          in1=T2[:, 0:h2], op=ALU.mult),
                         reads=[PB[ob], rl_bf[ri]], writes=[oT_bf[obuf]])
                    return
                if NORM_MODE == 0:
                    S.op("dve", lambda e: e.reciprocal(out=rl[ri][:, 0:ncols], in_=bank(ob)[64:128, 0:ncols]),
                         reads=[PB[ob]], writes=[rl_bf[ri]])
                elif NORM_MODE == 3:
                    S.op("act", lambda e: e.activation(out=rl[ri][:, 0:ncols], in_=bank(ob)[64:128, 0:ncols], func=AF.Copy),
                         reads=[PB[ob]], writes=[rl_bf[ri]])
                    S.op("pool", lambda e: e.tensor_tensor(out=rl[ri][:, 0:ncols], in0=rl[ri][:, 0:ncols], in1=mones[:, 0:ncols], op=ALU.pow),
                         reads=[rl_bf[ri], const_bf], writes=[rl_bf[ri]])
                else:
                    S.op("act", lambda e: e.activation(out=rl[ri][:, 0:ncols], in_=bank(ob)[64:128, 0:ncols], func=AF.Ln),
                         reads=[PB[ob]], writes=[rl_bf[ri]])
                    S.op("act", lambda e: e.activation(out=rl[ri][:, 0:ncols], in_=rl[ri][:, 0:ncols], func=AF.Exp, scale=-1.0),
                         reads=[rl_bf[ri]], writes=[rl_bf[ri]])
                S.op("dve", lambda e: e.tensor_tensor(out=oT[obuf][64 * hh:64 * hh + 64, col0:col0 + ncols],
                                                      in0=bank(ob)[0:64, 0:ncols], in1=rl[ri][:, 0:ncols], op=ALU.mult),
                     reads=[PB[ob], rl_bf[ri]], writes=[oT_bf[obuf]])

            def mm(out_ap, lhsT, rhs, start, stop, reads, wbuf):
                S.op("pe", lambda e: e.matmul(out_ap, lhsT=lhsT, rhs=rhs, start=start, stop=stop, skip_group_check=True),
                     reads=reads, writes=[wbuf])

            def dilated_head(pi, hh, obuf):
                qb, kb, qb_bf, kb_bf = qk_sets[pi % 2]
                slope = SLOPES_A[2 * pi + hh]
                r0, r1 = 64 * hh, 64 * hh + 64
                D1 = dtab[:, 0:512]
                for j in range(4):
                    ob = 5 + nxt("o", 2)
                    state = {"first": True}

                    def pv(out_ap, lhsT, rhs, rd, last=False, state=state, ob=ob):
                        mm(out_ap, lhsT, rhs, state["first"], last, rd, PB[ob])
                        state["first"] = False

                    for bi in range(2):
                        n0 = 4 * j + 2 * bi
                        c0 = 0 if n0 >= 1 else 128

                        def st_fn(bk, pbuf, kq, n0=n0):
                            if n0 >= 1:
                                mm(bk[:, 0:128], kb[r0:r1, 128 * (n0 - 1):128 * n0], qb[r0:r1, 128 * n0:128 * (n0 + 1)], True, True, kq, pbuf)
                            mm(bk[:, 128:384], kb[r0:r1, 128 * n0:128 * (n0 + 1)], qb[r0:r1, 128 * n0:128 * (n0 + 2)], True, True, kq, pbuf)
                            mm(bk[:, 384:512], kb[r0:r1, 128 * (n0 + 1):128 * (n0 + 2)], qb[r0:r1, 128 * (n0 + 1):128 * (n0 + 2)], True, True, kq, pbuf)

                        def pv_fn(pt, ptb, n0=n0, j=j, pv=pv, ob=ob):
                            for qi in range(2):
                                n = n0 + qi
                                o_ap = bank(ob)[:, 128 * (n - 4 * j):128 * (n - 4 * j + 1)]
                                if n >= 1:
                                    pv(o_ap, vaug[0][:, n - 1, hh, :], pt[:, 256 * qi:256 * qi + 128], [vaug_bf[0], ptb])
                                pv(o_ap, vaug[0][:, n, hh, :], pt[:, 256 * qi + 128:256 * qi + 256], [vaug_bf[0], ptb])

                        unit(st_fn, D1, (lambda ap, c0=c0: ap[:, c0:512]), -slope, 0.0, pv_fn, [qb_bf, kb_bf])
                    for bi in range(2):
                        def st_fn(bk, pbuf, kq, bi=bi, j=j):
                            for u in range(2):
                                r4 = 2 * bi + u
                                q_ap = qb[r0:r1, 512 * j + r4:512 * (j + 1):4]
                                if j >= 1:
                                    mm(bk[:, 256 * u:256 * u + 128], kb[r0:r1, 512 * (j - 1) + r4:512 * j:4], q_ap, True, True, kq, pbuf)
                                mm(bk[:, 256 * u + 128:256 * u + 256], kb[r0:r1, 512 * j + r4:512 * (j + 1):4], q_ap, True, True, kq, pbuf)

                        def pv_fn(pt, ptb, bi=bi, j=j, pv=pv, ob=ob):
                            for u in range(2):
                                r4 = 2 * bi + u
                                o_ap = bank(ob)[:, r4:512:4]
                                if j >= 1:
                                    pv(o_ap, vaug[1][:, 4 * r4 + j - 1, hh, :], pt[:, 256 * u:256 * u + 128], [vaug_bf[1], ptb])
                                pv(o_ap, vaug[1][:, 4 * r4 + j, hh, :], pt[:, 256 * u + 128:256 * u + 256], [vaug_bf[1], ptb])

                        if j >= 1:
                            cv = lambda ap: ap
                        else:
                            cv = lambda ap: ap.rearrange("p (u c) -> p u c", c=256)[:, :, 128:256]
                        unit(st_fn, D1, cv, -slope * 4.0, 0.0, pv_fn, [qb_bf, kb_bf])
                    def st_fn(bk, pbuf, kq, j=j):
                        for r16 in range(16):
                            mm(bk[:, 32 * r16:32 * (r16 + 1)], kb[r0:r1, r16:SEQ:16],
                               qb[r0:r1, 512 * j + r16:512 * (j + 1):16], True, True, kq, pbuf)

                    def pv_fn(pt, ptb, j=j, pv=pv, ob=ob):
                        for r16 in range(16):
                            pv(bank(ob)[:, r16:512:16], vaug[2][:, r16, hh, :], pt[:, 32 * r16:32 * (r16 + 1)],
                               [vaug_bf[2], ptb], last=(r16 == 15))

                    unit(st_fn, dtab[:, 512 * (1 + j):512 * (2 + j)], (lambda ap: ap), -slope * 16.0, 0.0, pv_fn, [qb_bf, kb_bf])
                    pend.append(lambda ob=ob, j=j: normalize(ob, 512, hh, obuf, 512 * j))

            def moba_head(pi, hh, obuf):
                qb, kb, qb_bf, kb_bf = qk_sets[pi % 2]
                hB = 2 * (pi - 6) + hh
                slope = SLOPES_B[hB]
                proj_qk(pi, 0, qb, qb_bf, rows=(64 * hh, 64 * hh + 64))
                proj_qk(pi, 1, kb, kb_bf, rows=(64 * hh, 64 * hh + 64))
                S.dma("pool", kb[64:74, :], erows_d[12 * hB:12 * hB + 10, :], writes=[kb_bf])
                S.dma("pool", qb[72:74, :], erows_d[12 * hB + 10:12 * hB + 12, :], writes=[qb_bf])
                S.op("dve", lambda e: e.tensor_reduce(out=kmf, in_=kb[0:64, :].rearrange("p (m t) -> p m t", t=256),
                                                      axis=mybir.AxisListType.X, op=ALU.add), reads=[kb_bf], writes=[misc_bf])
                S.op("dve", lambda e: e.tensor_copy(out=kmh, in_=kmf), reads=[misc_bf], writes=[misc_bf])
                S.op("dve", lambda e: e.tensor_tensor(out=kmd, in0=kmf, in1=kmh, op=ALU.subtract), reads=[misc_bf], writes=[misc_bf])
                S.op("dve", lambda e: e.tensor_copy(out=kml, in_=kmd), reads=[misc_bf], writes=[misc_bf])
                for t in range(NT):
                    mm(bank(7)[:, 8 * t:8 * t + 8], qb[0:64, 128 * t:128 * (t + 1)], kmh, True, False, [qb_bf, misc_bf], PB[7])
                    mm(bank(7)[:, 8 * t:8 * t + 8], qb[0:64, 128 * t:128 * (t + 1)], kml, False, True, [qb_bf, misc_bf], PB[7])
                S.op("dve", lambda e: e.tensor_tensor(out=gsb, in0=bank(7)[:, 0:128].rearrange("p (a m) -> p a m", m=8),
                                                      in1=gcon[:, 0:128].rearrange("p (a m) -> p a m", m=8), op=ALU.add),
                     reads=[PB[7], const_bf], writes=[misc_bf])
                for t in range(NT):
                    S.op("dve", lambda e, t=t: e.max(out=top8[:, t, :], in_=gsb[:, t, :]), reads=[misc_bf], writes=[misc_bf])
                S.op("dve", lambda e: e.tensor_scalar(out=thr, in0=top8[:, :, 2], scalar1=-1.0e29, scalar2=None, op0=ALU.max),
                     reads=[misc_bf], writes=[misc_bf])
                S.op("dve", lambda e: e.tensor_tensor(out=ind, in0=gsb, in1=thr.unsqueeze(2).broadcast_to([128, 16, 8]), op=ALU.is_ge),
                     reads=[misc_bf], writes=[misc_bf])
                S.op("dve", lambda e: e.tensor_tensor(out=ind, in0=ind, in1=gcon[:, 128:256].rearrange("p (a m) -> p a m", m=8), op=ALU.max),
                     reads=[misc_bf, const_bf], writes=[misc_bf])
                S.op("dve", lambda e: e.tensor_scalar(out=bsel, in0=ind, scalar1=-1.0, scalar2=-NEGB, op0=ALU.add, op1=ALU.mult),
                     reads=[misc_bf], writes=[misc_bf])
                psb = bank(7).bitcast(BF16)
                for half in range(2):
                    for t8 in range(8):
                        t = 8 * half + t8
                        S.op("pe", lambda e, t=t, t8=t8: e.transpose(out=psb[0:8, 128 * t8:128 * (t8 + 1)], in_=bsel[:, t, :], identity=ident[:]),
                             reads=[misc_bf, const_bf], writes=[PB[7]])
                    S.op("act", lambda e, half=half: e.activation(out=qb[64:72, 1024 * half:1024 * (half + 1)], in_=psb[0:8, :], func=AF.Copy),
                         reads=[PB[7]], writes=[qb_bf])
                DMP = dtab[:, 2560:3072]
                DMO = dtab[:, 3072:3584]
                for n in range(8):
                    ob = 5 + nxt("o", 2)
                    state = {"first": True}

                    def pv(out_ap, lhsT, rhs, rd, last=False, state=state, ob=ob):
                        mm(out_ap, lhsT, rhs, state["first"], last, rd, PB[ob])
                        state["first"] = False

                    for m in range(n + 1):
                        def st_fn(bk, pbuf, kq, n=n, m=m):
                            for h in range(2):
                                mm(bk[:, 256 * h:256 * (h + 1)], kb[0:74, 256 * m + 128 * h:256 * m + 128 * (h + 1)],
                                   qb[0:74, 256 * n:256 * (n + 1)], True, True, kq, pbuf)

                        def pv_fn(pt, ptb, n=n, m=m, pv=pv, ob=ob):
                            for h in range(2):
                                pv(bank(ob)[:, 0:256], vaug[0][:, 2 * m + h, hh, :], pt[:, 256 * h:256 * (h + 1)],
                                   [vaug_bf[0], ptb], last=(m == n and h == 1))

                        unit(st_fn, DMO if m == n else None, (lambda ap: ap), -1.0, -slope * 256.0 * (n - m), pv_fn, [qb_bf, kb_bf])
                    pend.append(lambda ob=ob, n=n: normalize(ob, 256, hh, obuf, 256 * n))

            def out_proj_tasks(pi, obuf):
                tasks = []

                def one(t):
                    for h in range(2):
                        S.op("pe", lambda e, h=h: e.matmul(
                            bank(h), lhsT=oT[obuf][:, 128 * t:128 * (t + 1)], rhs=wout_sl[pi % 2][:, 512 * h:512 * (h + 1)],
                            start=True, stop=True), reads=[oT_bf[obuf], wout_bf[pi % 2]], writes=[PB[h]])
                    S.op("dve", lambda e: e.scalar_tensor_tensor(
                        out=R[:, t, :], in0=bank(0, 2), scalar=C_MIX, in1=R[:, t, :], op0=ALU.mult, op1=ALU.add),
                        reads=[PB[0], PB[1], R_bf[t]], writes=[R_bf[t]])

                for t in range(NT):
                    tasks.append(lambda t=t: one(t))
                return tasks

            def attend(pi):
                obuf = pi % 2
                if pi < 6:
                    for hh in range(2):
                        dilated_head(pi, hh, obuf)
                else:
                    for hh in range(2):
                        moba_head(pi, hh, obuf)
                flush(0)
                filler_flush()

            def project(pi, qk_now=True):
                qb_, kb_, qbf_, kbf_ = qk_sets[pi % 2]
                if pi == 0:
                    load_w(pi)
                    proj_qk(pi, 0, qb_, qbf_, chunks=(0, 1, 2), pops=3)
                    proj_qk(pi, 1, kb_, kbf_, chunks=(0, 1, 2), pops=3)
                    bg_flush()
                    proj_qk(pi, 0, qb_, qbf_, chunks=(3,))
                    proj_qk(pi, 1, kb_, kbf_, chunks=(3,))
                    proj_v(pi, (0, 1, 2))
                elif pi < 6:
                    proj_v(pi, (0, 1, 2))
                else:
                    proj_v(pi, (0,))

            def queue_qk(pi):
                load_w(pi)
                if pi < 6:
                    qb_, kb_, qbf_, kbf_ = qk_sets[pi % 2]
                    proj_qk(pi, 0, qb_, qbf_, defer=fillers)
                    proj_qk(pi, 1, kb_, kbf_, defer=fillers)

            load_wout(0)
            project(0)
            for pi in range(8):
                if pi + 1 < 8:
                    queue_qk(pi + 1)
                attend(pi)
                if pi + 1 < 8:
                    load_wout(pi + 1)
                    project(pi + 1)
                fillers.extend(out_proj_tasks(pi, pi % 2))
            filler_flush()
            for g in range(4):
                bg.extend(ln_tasks([4 * g + tt for tt in range(4)], post_ln2))
                if g == 0:
                    bg_flush()

        def post_ln2(t):
            def a():
                if dbg:
                    S.dma("pool", dbg_h2[128 * t:128 * (t + 1), :], R[:, t, :], reads=[R_bf[t]], writes=[out_bf])
            return [a]

        cast_ffn(0)
        for s in range(n_seq):
            ffn_phase(s, 0, 0, True)
            if s == 0:
                cast_attn()
                cast_ffn(1)
            attn_phase(s)
            ffn_phase(s, 1, 2, False)
        bg_flush()
        S.emit(block)
    return nc


_PROGRAM_CACHE = {}


def _layout_weights(inp):
    def gu(gate, up):
        def r(w):
            return w.reshape(KC, 128, FC, 128).transpose(2, 1, 0, 3)
        a = np.stack([r(gate), r(up)], axis=2)
        return np.ascontiguousarray(a.reshape(FC * 128, 2048))

    def dn(w):
        return np.ascontiguousarray(w.reshape(FC, 128, DM).transpose(1, 0, 2).reshape(128, FC * DM))

    w_in = inp["w_in"][0]
    blocks = []
    w3 = w_in.reshape(KC, 128, 3072)
    for pi in range(8):
        if pi < 6:
            cols = (128 * pi, 768 + 128 * pi, 1536 + 128 * pi)
        else:
            cols = (2304 + 128 * (pi - 6), 2560 + 128 * (pi - 6), 2816 + 128 * (pi - 6))
        per_t = [w3[:, :, c:c + 128].transpose(1, 0, 2) for c in cols]
        blocks.append(np.stack(per_t, axis=1).reshape(128, 3072))
    win = np.ascontiguousarray(np.concatenate(blocks, axis=0))
    ln = np.ascontiguousarray(np.stack([inp["ln1_g"][0], inp["ln1_b"][0], inp["ln2_g"][0], inp["ln2_b"][0],
                                        inp["ln3_g"][0], inp["ln3_b"][0]], axis=0))
    return {
        "wgu1": gu(inp["ffn1_gate"][0], inp["ffn1_up"][0]),
        "wgu2": gu(inp["ffn2_gate"][0], inp["ffn2_up"][0]),
        "wd1": dn(inp["ffn1_down"][0]),
        "wd2": dn(inp["ffn2_down"][0]),
        "win": win,
        "wout": np.ascontiguousarray(inp["w_out"][0]),
        "ln": ln,
    }


def kernel(**inputs):
    inp = {k: np.asarray(v, dtype=np.float32) for k, v in inputs.items()}
    x = inp["x"]
    assert x.shape == (BATCH, SEQ, DM)
    shared = _layout_weights(inp)
    dtab, gcon, erows, ident = _const_tables()
    shared.update({"dtab": dtab, "gcon": gcon, "erows": erows, "ident": ident})
    if "nc" not in _PROGRAM_CACHE:
        _PROGRAM_CACHE["nc"] = build_program(BPC)
    nc = _PROGRAM_CACHE["nc"]
    in_maps = []
    for c in range(NCORES):
        m = dict(shared)
        m["x"] = np.ascontiguousarray(x[c * BPC:(c + 1) * BPC].reshape(BPC * SEQ, DM))
        in_maps.append(m)
    res = run_bass_kernel_spmd(nc, in_maps, core_ids=list(range(NCORES)))
    outs = [np.asarray(r["out"]).reshape(BPC, SEQ, DM) for r in res.results]
    return np.concatenate(outs, axis=0).astype(np.float32)
```

```python
import math
from contextlib import ExitStack

import numpy as np
import ml_dtypes

import concourse.bass as bass
import concourse.mybir as mybir
from concourse.bass_utils import run_bass_kernel_spmd

F32 = mybir.dt.float32
BF16 = mybir.dt.bfloat16
AF = mybir.ActivationFunctionType
ALU = mybir.AluOpType

NCORES = 8


class Buf:
    __slots__ = ("name", "psum", "writers", "readers")

    def __init__(self, name, psum=False):
        self.name = name
        self.psum = psum
        self.writers = []
        self.readers = []


class Op:
    __slots__ = ("eng", "fn", "deps", "is_dma", "sem", "val", "marked", "idx", "prev_lane")

    def __init__(self, eng, fn, is_dma=False):
        self.eng = eng
        self.fn = fn
        self.deps = []
        self.is_dma = is_dma
        self.sem = None
        self.val = None
        self.marked = False
        self.idx = None
        self.prev_lane = None


ENGINES = ("pe", "act", "dve", "pool", "sp")


class Sched:
    def __init__(self, nc, es, lanes_sp=8, lanes_pool=6):
        self.nc = nc
        self.ops = {e: [] for e in ENGINES}
        self.esem = {e: es.enter_context(nc.semaphore("sem_" + e)) for e in ENGINES}
        self.lanes = {
            "sp": [es.enter_context(nc.semaphore(f"dsp{i}")) for i in range(lanes_sp)],
            "pool": [es.enter_context(nc.semaphore(f"dpl{i}")) for i in range(lanes_pool)],
        }
        self.lane_rr = {"sp": 0, "pool": 0}
        self.lane_val = {}
        self.lane_last = {}

    @staticmethod
    def _merge(lst, op):
        if not op.is_dma:
            lst[:] = [o for o in lst if o.is_dma or o.eng != op.eng]
        lst.append(op)

    def _track(self, op, reads, writes):
        deps = op.deps
        for b in reads:
            for w in b.writers:
                deps.append(w)
            if b.psum:
                for r in b.readers:
                    if r.eng != op.eng:
                        deps.append(r)
        for b in writes:
            for w in b.writers:
                if w.is_dma or op.is_dma or w.eng != op.eng:
                    deps.append(w)
            for r in b.readers:
                if r.is_dma or op.is_dma or r.eng != op.eng:
                    deps.append(r)
        for b in reads:
            self._merge(b.readers, op)
        for b in writes:
            b.writers = [op]
            b.readers = []
        for d in deps:
            d.marked = True

    def op(self, eng, fn, reads=(), writes=()):
        o = Op(eng, fn)
        self._track(o, reads, writes)
        self.ops[eng].append(o)
        return o

    def dma(self, q, out, in_, reads=(), writes=()):
        o = Op(q, lambda e: e.dma_start(out=out, in_=in_), is_dma=True)
        lanes = self.lanes[q]
        li = self.lane_rr[q]
        self.lane_rr[q] = (li + 1) % len(lanes)
        sem = lanes[li]
        o.sem = sem
        o.val = self.lane_val.get(id(sem), 0) + 16
        self.lane_val[id(sem)] = o.val
        o.prev_lane = self.lane_last.get(id(sem))
        self.lane_last[id(sem)] = o
        self._track(o, reads, writes)
        self.ops[q].append(o)
        return o

    def emit(self, block):
        nc = self.nc
        for e in ENGINES:
            c = 0
            for o in self.ops[e]:
                if not o.is_dma:
                    if o.marked:
                        c += 1
                        o.sem = self.esem[e]
                        o.val = c
        final_waits = []
        for q in ("sp", "pool"):
            for sem in self.lanes[q]:
                last = self.lane_last.get(id(sem))
                if last is not None:
                    final_waits.append((sem, last.val))

        def run(e, eng):
            waited = {}
            for o in self.ops[e]:
                need = {}
                deps = list(o.deps)
                if o.is_dma and o.prev_lane is not None:
                    deps.append(o.prev_lane)
                for d in deps:
                    k = id(d.sem)
                    if waited.get(k, 0) >= d.val:
                        continue
                    if k not in need or need[k][1] < d.val:
                        need[k] = (d.sem, d.val)
                for k, (sem, val) in need.items():
                    eng.wait_ge(sem, val)
                    waited[k] = val
                inst = o.fn(eng)
                if o.is_dma:
                    inst.then_inc(o.sem, 16)
                elif o.marked:
                    inst.then_inc(o.sem, 1)
            if e == "sp":
                for sem, val in final_waits:
                    eng.wait_ge(sem, val)

        @block.tensor
        def _(eng):
            run("pe", eng)

        @block.scalar
        def _(eng):
            run("act", eng)

        @block.vector
        def _(eng):
            run("dve", eng)

        @block.gpsimd
        def _(eng):
            run("pool", eng)

        @block.sync
        def _(eng):
            run("sp", eng)


SEQ = 2048
DM = 1024
DFF = 2816
NT = SEQ // 128
KC = DM // 128
FC = DFF // 128
BATCH = 32
BPC = BATCH // NCORES
ALPHA = 2.0 ** 0.25
EPS_LN = 1e-5 / (ALPHA * ALPHA)
C_FFN = 0.5 / ALPHA
C_MIX = 1.0 / ALPHA
FGROUPS = [(0, 6), (6, 12), (12, 17), (17, 22)]
BIG = 1.0e9
NEGB = -30000.0
NORM_MODE = 0

_idx = np.arange(16)
_sl = 2.0 ** (-8.0 * (_idx + 1) / 16)
SLOPES_A = [float(v) for v in _sl[(_idx % 4) != 3]]
SLOPES_B = [float(v) for v in _sl[(_idx % 4) == 3]]


def _const_tables():
    k = np.arange(128)[:, None].astype(np.float64)
    q = np.arange(128)[None, :].astype(np.float64)
    own = np.where(q >= k, q - k, BIG)
    prev = np.where(q <= k, 128 + q - k, BIG)
    d1 = np.concatenate([prev, own, prev, own], axis=1)
    d3 = []
    for j in range(4):
        c = (32 * j + np.arange(32))[None, :].astype(np.float64)
        u = np.where(c >= k, c - k, BIG)
        d3.append(np.tile(u, (1, 16)))
    q2 = np.arange(256)[None, :].astype(np.float64)
    dmp = np.concatenate([q2 - k, q2 - 128 - k], axis=1)
    dmo = np.where(dmp >= 0, 0.0, BIG)
    dtab = np.concatenate([d1] + d3 + [dmp, dmo], axis=1)
    neg = np.zeros((128, 16, 8), np.float32)
    ownb = np.zeros((128, 16, 8), np.float32)
    for t in range(16):
        n = t // 2
        neg[:, t, n:] = -1.0e30
        ownb[:, t, n] = 1.0
    gcon = np.concatenate([neg.reshape(128, 128), ownb.reshape(128, 128)], axis=1)
    loc = (np.arange(SEQ) % 256).astype(np.float32)
    erows = np.zeros((4, 12, SEQ), np.float32)
    for hb in range(4):
        for m in range(8):
            erows[hb, m, 256 * m:256 * (m + 1)] = 1.0
        erows[hb, 8] = 1.0
        erows[hb, 9] = SLOPES_B[hb] * loc
        erows[hb, 10] = -SLOPES_B[hb] * loc
        erows[hb, 11] = 1.0
    erows = erows.reshape(48, SEQ)
    return (dtab.astype(ml_dtypes.bfloat16), gcon.astype(np.float32),
            erows.astype(ml_dtypes.bfloat16), np.eye(128).astype(ml_dtypes.bfloat16))


def build_program(n_seq=BPC, dbg=False):
    nc = bass.Bass("TRN2", target_bir_lowering=False)

    def dram(name, shape, dtype, kind):
        return nc.dram_tensor(name, shape, dtype, kind=kind).ap()

    x = dram("x", [n_seq * SEQ, DM], F32, "ExternalInput")
    out = dram("out", [n_seq * SEQ, DM], F32, "ExternalOutput")
    wgu_f = [dram(f"wgu{i}", [FC * 128, 2048], F32, "ExternalInput") for i in (1, 2)]
    wd_f = [dram(f"wd{i}", [128, FC * 1024], F32, "ExternalInput") for i in (1, 2)]
    win_f = dram("win", [8 * 128, 3072], F32, "ExternalInput")
    wout_f = dram("wout", [8 * 128, 1024], F32, "ExternalInput")
    ln_d = dram("ln", [6, DM], F32, "ExternalInput")
    dtab_d = dram("dtab", [128, 3584], BF16, "ExternalInput")
    gcon_d = dram("gcon", [128, 256], F32, "ExternalInput")
    erows_d = dram("erows", [48, SEQ], BF16, "ExternalInput")
    ident_d = dram("ident", [128, 128], BF16, "ExternalInput")
    wgu_b = [dram(f"wgu{i}b", [FC * 128, 2048], BF16, "Internal") for i in (1, 2)]
    wd_b = [dram(f"wd{i}b", [128, FC * 1024], BF16, "Internal") for i in (1, 2)]
    win_b = dram("winb", [8 * 128, 3072], BF16, "Internal")
    wout_b = dram("woutb", [8 * 128, 1024], BF16, "Internal")
    if dbg:
        dbg_h1 = dram("dbg_h1", [SEQ, DM], F32, "ExternalOutput")
        dbg_h2 = dram("dbg_h2", [SEQ, DM], F32, "ExternalOutput")

    es = ExitStack()
    with es:
        S = Sched(nc, es, lanes_sp=10, lanes_pool=30)

        def sb(name, shape, dtype):
            return es.enter_context(nc.sbuf_tensor(name, shape, dtype))

        R = sb("R", [128, NT, DM], F32)
        hT = sb("hT", [128, KC, SEQ], BF16)
        lnp_s = [sb(f"lnp{i}", [128, 2, DM], F32) for i in range(2)]
        ident = sb("ident_sb", [128, 128], BF16)
        dtab = sb("dtab_sb", [128, 3584], BF16)
        gcon = sb("gcon_sb", [128, 256], F32)
        mones = sb("mones", [64, 512], F32)
        stt_s = [sb(f"stt{i}", [128, 4, 2, 6], F32) for i in range(2)]
        mv_s = [sb(f"mv{i}", [128, 4, 2], F32) for i in range(2)]
        rs_s = [sb(f"rs{i}", [128, 3, 4], F32) for i in range(2)]
        nbias_s = [sb(f"nbias{i}", [128, 4], F32) for i in range(2)]
        UEL = 40960
        U = sb("U", [128, UEL], BF16)
        ps = es.enter_context(nc.psum_tensor("ps", [128, 4096], F32))
        block = es.enter_context(nc.Block())

        def bank(i, n=1):
            return ps[:, 512 * i:512 * (i + n)]

        PB = [Buf(f"bank{i}", psum=True) for i in range(8)]

        arena = []

        class ABuf(Buf):
            __slots__ = ("lo", "hi", "over")

            def __init__(self, name, lo, hi):
                super().__init__(name)
                self.lo, self.hi = lo, hi
                self.over = []
                for o in arena:
                    if o.lo < hi and lo < o.hi:
                        o.over.append(self)
                        self.over.append(o)
                arena.append(self)

        _orig_track = S._track

        def _track(op, reads, writes):
            for b in writes:
                if isinstance(b, ABuf):
                    for ob in b.over:
                        for w in ob.writers + ob.readers:
                            if w.is_dma or op.is_dma or w.eng != op.eng:
                                op.deps.append(w)
            for b in reads:
                if isinstance(b, ABuf):
                    for ob in b.over:
                        for w in ob.writers:
                            op.deps.append(w)
            _orig_track(op, reads, writes)

        S._track = _track

        def uview(lo, n, dtype=BF16, parts=128):
            ap = U[0:parts, lo:lo + n]
            if dtype == F32:
                ap = ap.bitcast(F32)
            return ap

        wd_sl = [uview(6144 * i, 6144).rearrange("p (f d) -> p f d", d=1024) for i in range(2)]
        wd_bf = [ABuf(f"wd_sl{i}", 6144 * i, 6144 * (i + 1)) for i in range(2)]
        act_sl = [uview(12288 + 3072 * i, 3072).rearrange("p (f t) -> p f t", t=512) for i in range(2)]
        act_bf = [ABuf(f"act{i}", 12288 + 3072 * i, 12288 + 3072 * (i + 1)) for i in range(2)]
        wgu_sl = [uview(18432 + 2048 * i, 2048).rearrange("p (g k f) -> p g k f", g=2, k=8) for i in range(3)]
        wgu_bf = [ABuf(f"wgu{i}", 18432 + 2048 * i, 18432 + 2048 * (i + 1)) for i in range(3)]
        hTs = uview(24576, 4096).rearrange("p (k t) -> p k t", t=512)
        hTs_bf = ABuf("hTs", 24576, 28672)
        _xo = [28672, 29696, 32768, 33792]
        xb = [uview(o, 1024) for o in _xo]
        xb_bf = [ABuf(f"xb{i}", o, o + 1024) for i, o in enumerate(_xo)]
        sg = [uview(30720 + 1024 * i, 1024, F32) for i in range(2)]
        sg_bf = [ABuf(f"sg{i}", 30720 + 1024 * i, 30720 + 1024 * (i + 1)) for i in range(2)]
        win_sl = [uview(3072 * i, 3072).rearrange("p (t k c) -> p t k c", t=3, k=8) for i in range(2)]
        win_bf = [ABuf(f"win{i}", 3072 * i, 3072 * (i + 1)) for i in range(2)]
        wout_sl = [uview(6144 + 1024 * i, 1024) for i in range(2)]
        wout_bf = [ABuf(f"wout{i}", 6144 + 1024 * i, 6144 + 1024 * (i + 1)) for i in range(2)]
        qb = uview(8192, 2048)
        kb = uview(10240, 2048)
        qb_bf = ABuf("qb", 8192, 10240)
        kb_bf = ABuf("kb", 10240, 12288)
        qk_sets = [(qb, kb, qb_bf, kb_bf),
                   (uview(36864, 2048), uview(38912, 2048), ABuf("qb2", 36864, 38912), ABuf("kb2", 38912, 40960))]
        vaug = [uview(12288 + 4096 * i, 4096).rearrange("p (a h c) -> p a h c", h=2, c=128) for i in range(3)]
        vaug_bf = [ABuf(f"vaug{i}", 12288 + 4096 * i, 12288 + 4096 * (i + 1)) for i in range(3)]
        oT = [uview(24576 + 2048 * i, 2048) for i in range(2)]
        oT_bf = [ABuf(f"oT{i}", 24576 + 2048 * i, 24576 + 2048 * (i + 1)) for i in range(2)]
        pT = [uview(28672 + 512 * i, 512) for i in range(4)]
        pT_bf = [ABuf(f"pT{i}", 28672 + 512 * i, 28672 + 512 * (i + 1)) for i in range(4)]
        sbias = [uview(30720 + 1024 * i, 1024, F32) for i in range(3)]
        sbias_bf = [ABuf(f"sbias{i}", 30720 + 1024 * i, 30720 + 1024 * (i + 1)) for i in range(3)]
        rl = [uview(33792 + 1024 * i, 1024, F32, parts=64) for i in range(2)]
        rlT = [uview(33792 + 1024 * i, 512, F32) for i in range(2)]
        rl2 = [uview(33792 + 1024 * i + 512, 512, F32, parts=64) for i in range(2)]
        rl_bf = [ABuf(f"rl{i}", 33792 + 1024 * i, 33792 + 1024 * (i + 1)) for i in range(2)]
        gsb = uview(35840, 256, F32).rearrange("p (a m) -> p a m", m=8)
        top8 = uview(36096, 256, F32).rearrange("p (a m) -> p a m", m=8)
        thr = uview(36352, 32, F32)
        ind = uview(36384, 256, F32).rearrange("p (a m) -> p a m", m=8)
        bsel = uview(36640, 128).rearrange("p (a m) -> p a m", m=8)
        kmf = uview(36768, 16, F32, parts=64)
        kmh = uview(36784, 8, parts=64)
        kml = uview(36792, 8, parts=64)
        kmd = uview(36800, 16, F32, parts=64)
        misc_bf = ABuf("moba_misc", 35840, 36864)

        R_bf = [Buf(f"R{t}") for t in range(NT)]
        hT_bf = [Buf(f"hT{t}") for t in range(NT)]
        lnp_bfs = [Buf("lnp0"), Buf("lnp1")]
        ln_state = {"cur": 0}

        def load_ln(ln_idx):
            b = (ln_state["cur"] + 1) % 2
            ln_state["cur"] = b
            bg_require([("lnp", b)])
            S.dma("pool", lnp_s[b][:], ln_d[2 * ln_idx:2 * ln_idx + 2, :].partition_broadcast(128), writes=[lnp_bfs[b]])
        const_bf = Buf("const")
        st_bfs = [Buf("stats0"), Buf("stats1")]
        import collections as _c
        bg = _c.deque()

        def bg_pop(k=1):
            for _ in range(k):
                if bg:
                    bg.popleft()[1]()

        def bg_flush():
            while bg:
                bg.popleft()[1]()

        def bg_require(tiles):
            tiles = set(tiles)
            last = -1
            for i, (tg, _) in enumerate(bg):
                if tg & tiles:
                    last = i
            for _ in range(last + 1):
                bg.popleft()[1]()
        out_bf = Buf("out_dram")
        wgub_bf = [[Buf(f"wgub{i}_{f}") for f in range(FC)] for i in range(2)]
        wdb_bf = [[Buf(f"wdb{i}_{g}") for g in range(4)] for i in range(2)]
        winb_bf = [Buf(f"winb{p}") for p in range(8)]
        woutb_bf = [Buf(f"woutb{p}") for p in range(8)]

        rot = {"gu": 0, "wgu": 0, "y": 0, "sg": 0, "xb": 0, "proj": 0, "st": 0, "pt": 0, "sb": 0,
               "o": 0, "rl": 0, "ybo": 0, "lnset": 0}

        def nxt(k, n):
            v = rot[k]
            rot[k] = (v + 1) % n
            return v

        S.dma("sp", ident[:], ident_d, writes=[const_bf])
        S.dma("sp", dtab[:], dtab_d, writes=[const_bf])
        S.dma("sp", gcon[:], gcon_d, writes=[const_bf])
        S.op("dve", lambda e: e.memset(mones[:], -1.0), writes=[const_bf])

        def cast_ffn(i):
            for f in range(FC):
                S.dma("pool", wgu_b[i][128 * f:128 * (f + 1), :], wgu_f[i][128 * f:128 * (f + 1), :],
                      writes=[wgub_bf[i][f]])
                if f in (5, 11, 16, 21):
                    g = (5, 11, 16, 21).index(f)
                    f0, f1 = FGROUPS[g]
                    S.dma("pool", wd_b[i][:, 1024 * f0:1024 * f1], wd_f[i][:, 1024 * f0:1024 * f1],
                          writes=[wdb_bf[i][g]])

        def cast_attn():
            for p in range(8):
                S.dma("pool", win_b[128 * p:128 * (p + 1), :], win_f[128 * p:128 * (p + 1), :], writes=[winb_bf[p]])
                S.dma("pool", wout_b[128 * p:128 * (p + 1), :], wout_f[128 * p:128 * (p + 1), :], writes=[woutb_bf[p]])

        def ln_tasks(tiles, post):
            n = len(tiles)
            si_ = nxt("lnset", 2)
            stt, mv, rs, nbias, st_bf = stt_s[si_], mv_s[si_], rs_s[si_], nbias_s[si_], st_bfs[si_]
            lb = ln_state["cur"]
            lnp, lnp_bf = lnp_s[lb], lnp_bfs[lb]
            tasks = []

            def stats(i, t):
                for h in range(2):
                    S.op("dve", lambda e, h=h: e.bn_stats(out=stt[:, i, h, :], in_=R[:, t, 512 * h:512 * (h + 1)]),
                         reads=[R_bf[t]], writes=[st_bf])
                S.op("dve", lambda e: e.bn_aggr(out=mv[:, i, :], in_=stt[:, i, :, :]), reads=[st_bf], writes=[st_bf])

            def chain():
                S.op("dve", lambda e: e.tensor_scalar(out=rs[:, 0, 0:n], in0=mv[:, 0:n, 1], scalar1=EPS_LN, scalar2=None, op0=ALU.add),
                     reads=[st_bf], writes=[st_bf])
                S.op("act", lambda e: e.activation(out=rs[:, 1, 0:n], in_=rs[:, 0, 0:n], func=AF.Sqrt), reads=[st_bf], writes=[st_bf])
                S.op("dve", lambda e: e.reciprocal(out=rs[:, 2, 0:n], in_=rs[:, 1, 0:n]), reads=[st_bf], writes=[st_bf])
                S.op("dve", lambda e: e.scalar_tensor_tensor(out=nbias[:, 0:n], in0=mv[:, 0:n, 0], scalar=-1.0, in1=rs[:, 2, 0:n],
                                                             op0=ALU.mult, op1=ALU.mult), reads=[st_bf], writes=[st_bf])

            def norm(i, t):
                S.op("act", lambda e: e.activation(out=R[:, t, :], in_=R[:, t, :], func=AF.Identity,
                                                   scale=rs[:, 2, i:i + 1], bias=nbias[:, i:i + 1]),
                     reads=[R_bf[t], st_bf], writes=[R_bf[t]])
                S.op("dve", lambda e: e.tensor_tensor(out=R[:, t, :], in0=R[:, t, :], in1=lnp[:, 0, :], op=ALU.mult),
                     reads=[R_bf[t], lnp_bf], writes=[R_bf[t]])
                S.op("dve", lambda e: e.tensor_tensor(out=R[:, t, :], in0=R[:, t, :], in1=lnp[:, 1, :], op=ALU.add),
                     reads=[R_bf[t], lnp_bf], writes=[R_bf[t]])

            for i, t in enumerate(tiles):
                tasks.append(({t}, lambda i=i, t=t: stats(i, t)))
            tasks.append((set(tiles), chain))
            slots = {}
            for i, t in enumerate(tiles):
                slots.setdefault(2 * i, []).append(({t, ("lnp", lb)}, lambda i=i, t=t: norm(i, t)))
                for k, fn in enumerate(post(t)):
                    slots.setdefault(2 * i + 3 * (k + 1), []).append(({t}, fn))
            for k in sorted(slots):
                tasks.extend(slots[k])
            return tasks

        def cast_bf16(t):
            xi = nxt("xb", 4)
            S.op("act", lambda e: e.activation(out=xb[xi], in_=R[:, t, :], func=AF.Copy), reads=[R_bf[t]], writes=[xb_bf[xi]])
            return xi

        def transpose_to(xi, dst, dst_bf, col0):
            psb = bank(7).bitcast(BF16)
            for kc in range(KC):
                S.op("pe", lambda e, kc=kc: e.transpose(out=psb[:, 128 * kc:128 * (kc + 1)], in_=xb[xi][:, 128 * kc:128 * (kc + 1)],
                                                         identity=ident[:]),
                     reads=[xb_bf[xi], const_bf], writes=[PB[7]])
            S.op("act", lambda e: e.activation(out=dst[:, :, col0:col0 + 128],
                                               in_=psb.rearrange("p (k t) -> p k t", t=128), func=AF.Copy),
                 reads=[PB[7]], writes=[dst_bf])

        def ffn_phase(s, wi, ln_idx, first):
            load_ln(ln_idx)

            def load_x(sbk):
                bg_require(range(4 * sbk, 4 * sbk + 4))
                for tt in range(4):
                    t = 4 * sbk + tt
                    r0 = s * SEQ + 128 * t
                    S.dma("sp", R[:, t, :], x[r0:r0 + 128, :], writes=[R_bf[t]])

            prep_slots = {}

            def prep_cast(sbk):
                bg_require(range(4 * sbk, 4 * sbk + 4))
                prep_slots[sbk] = [cast_bf16(4 * sbk + tt) for tt in range(4)]

            def prep(sbk):
                for tt in range(4):
                    transpose_to(prep_slots[sbk][tt], hTs, hTs_bf, 128 * tt)

            def load_wd(g):
                f0, f1 = FGROUPS[g]
                S.dma("sp", wd_sl[g % 2][:, 0:f1 - f0, :],
                      wd_b[wi][:, 1024 * f0:1024 * f1].rearrange("p (f d) -> p f d", d=1024),
                      reads=[wdb_bf[wi][g]], writes=[wd_bf[g % 2]])

            def gate_up(g):
                f0, f1 = FGROUPS[g]
                for f in range(f0, f1):
                    ws = nxt("wgu", 3)
                    S.dma("sp", wgu_sl[ws], wgu_b[wi][128 * f:128 * (f + 1), :].rearrange("p (g k f) -> p g k f", g=2, k=8),
                          reads=[wgub_bf[wi][f]], writes=[wgu_bf[ws]])
                    banks = []
                    for gu in range(2):
                        b = nxt("gu", 3)
                        banks.append(b)
                        for kc in range(KC):
                            S.op("pe", lambda e, b=b, gu=gu, kc=kc, ws=ws: e.matmul(
                                bank(b), lhsT=wgu_sl[ws][:, gu, kc, :], rhs=hTs[:, kc, :], start=(kc == 0), stop=(kc == KC - 1)),
                                reads=[wgu_bf[ws], hTs_bf], writes=[PB[b]])
                    si = nxt("sg", 2)
                    S.op("act", lambda e, b=banks[0], si=si: e.activation(out=sg[si], in_=bank(b), func=AF.Silu),
                         reads=[PB[banks[0]]], writes=[sg_bf[si]])
                    S.op("dve", lambda e, b=banks[1], si=si, g=g, fl=f - f0: e.tensor_tensor(
                        out=act_sl[g % 2][:, fl, :], in0=sg[si], in1=bank(b), op=ALU.mult),
                        reads=[sg_bf[si], PB[banks[1]]], writes=[act_bf[g % 2]])
                    bg_pop(2)

            def down(sbk, g):
                f0, f1 = FGROUPS[g]
                nf = f1 - f0
                for tt in range(4):
                    t = 4 * sbk + tt
                    yb = 3 + 2 * nxt("y", 2)
                    for fl in range(nf):
                        for h in range(2):
                            S.op("pe", lambda e, fl=fl, h=h, yb=yb, tt=tt: e.matmul(
                                bank(yb + h), lhsT=act_sl[g % 2][:, fl, 128 * tt:128 * (tt + 1)],
                                rhs=wd_sl[g % 2][:, fl, 512 * h:512 * (h + 1)], start=(fl == 0), stop=(fl == nf - 1)),
                                reads=[act_bf[g % 2], wd_bf[g % 2]], writes=[PB[yb + h]])
                    S.op("dve", lambda e, yb=yb, t=t: e.scalar_tensor_tensor(
                        out=R[:, t, :], in0=bank(yb, 2), scalar=C_FFN, in1=R[:, t, :], op0=ALU.mult, op1=ALU.add),
                        reads=[PB[yb], PB[yb + 1], R_bf[t]], writes=[R_bf[t]])

            def post1(t):
                st = {}

                def a():
                    st["xi"] = cast_bf16(t)
                    if dbg:
                        S.dma("pool", dbg_h1[128 * t:128 * (t + 1), :], R[:, t, :], reads=[R_bf[t]], writes=[out_bf])

                def b():
                    transpose_to(st["xi"], hT, hT_bf[t], 128 * t)
                return [a, b]

            def post2(t):
                def a():
                    r0 = s * SEQ + 128 * t
                    S.dma("pool", out[r0:r0 + 128, :], R[:, t, :], reads=[R_bf[t]], writes=[out_bf])
                return [a]

            if first:
                load_x(0)
            prep_cast(0)
            prep(0)
            for sbk in range(4):
                if first and sbk + 1 < 4:
                    load_x(sbk + 1)
                for g in range(4):
                    load_wd(g)
                    if g == 3 and sbk + 1 < 4:
                        prep_cast(sbk + 1)
                    gate_up(g)
                    if g == 3 and sbk + 1 < 4:
                        prep(sbk + 1)
                    down(sbk, g)
                bg.extend(ln_tasks([4 * sbk + tt for tt in range(4)], post1 if first else post2))

        def attn_phase(s):
            load_ln(1)
            for o in range(3):
                for hh in range(2):
                    S.op("dve", lambda e, o=o, hh=hh: e.memset(vaug[o][:, :, hh, 64:128], 1.0), writes=[vaug_bf[o]])

            def load_w(pi):
                S.dma("sp", win_sl[pi % 2], win_b[128 * pi:128 * (pi + 1), :].rearrange("p (t k c) -> p t k c", t=3, k=8),
                      reads=[winb_bf[pi]], writes=[win_bf[pi % 2]])
                S.dma("sp", wout_sl[pi % 2], wout_b[128 * pi:128 * (pi + 1), :], reads=[woutb_bf[pi]], writes=[wout_bf[pi % 2]])

            hT_all = list(hT_bf)

            def proj_qk(pi, t, dst, dst_bf, rows=None, chunks=(0, 1, 2, 3), pops=0, defer=None):
                w = win_sl[pi % 2]

                def one(c):
                    b = nxt("proj", 2)
                    for kc in range(KC):
                        if rows is None:
                            lhsT = w[:, t, kc, :]
                            o_ap = bank(b)
                        else:
                            lhsT = w[:, t, kc, rows[0]:rows[1]]
                            o_ap = bank(b)[0:64, :]
                        S.op("pe", lambda e, lhsT=lhsT, o_ap=o_ap, kc=kc, c=c: e.matmul(
                            o_ap, lhsT=lhsT, rhs=hT[:, kc, 512 * c:512 * (c + 1)], start=(kc == 0), stop=(kc == KC - 1)),
                            reads=[win_bf[pi % 2]] + hT_all[4 * c:4 * c + 4], writes=[PB[b]])
                    np_ = 128 if rows is None else 64
                    S.op("act", lambda e, b=b, c=c, np_=np_: e.activation(
                        out=dst[0:np_, 512 * c:512 * (c + 1)], in_=bank(b)[0:np_, :], func=AF.Copy,
                        scale=(0.125 if t == 0 else 1.0)),
                        reads=[PB[b]], writes=[dst_bf])

                for c in chunks:
                    if defer is not None:
                        defer.append(lambda c=c: one(c))
                    else:
                        bg_pop(pops)
                        one(c)

            def tok_ap(o, tile, kc):
                if o == 0:
                    return hT[:, kc, 128 * tile:128 * (tile + 1)]
                if o == 1:
                    r4, j = tile // 4, tile % 4
                    return hT[:, kc, 512 * j + r4:512 * (j + 1):4]
                return hT[:, kc, tile:SEQ:16]

            def proj_v(pi, orders):
                w = win_sl[pi % 2]
                for o in orders:
                    for grp in range(4):
                        b = nxt("proj", 2)
                        for ti in range(4):
                            tile = 4 * grp + ti
                            for kc in range(KC):
                                S.op("pe", lambda e, b=b, ti=ti, tile=tile, kc=kc, o=o: e.matmul(
                                    bank(b)[:, 128 * ti:128 * (ti + 1)], lhsT=tok_ap(o, tile, kc), rhs=w[:, 2, kc, :],
                                    start=(kc == 0), stop=(kc == KC - 1), skip_group_check=True),
                                    reads=[win_bf[pi % 2]] + hT_all, writes=[PB[b]])
                        S.op("act", lambda e, b=b, grp=grp, o=o: e.activation(
                            out=vaug[o][:, 4 * grp:4 * grp + 4, :, 0:64],
                            in_=bank(b).rearrange("p (a h c) -> p a h c", h=2, c=64), func=AF.Copy),
                            reads=[PB[b]], writes=[vaug_bf[o]])

            pend = []
            LAG = 3
            ST_BANKS = (2, 3, 4, 7)
            fillers = []
            ucount = {"n": 0}

            def filler_pop(force=False):
                ucount["n"] += 1
                if fillers and (force or ucount["n"] % 8 == 0):
                    fillers.pop(0)()

            def filler_flush():
                while fillers:
                    fillers.pop(0)()

            def flush(k):
                while len(pend) > k:
                    pend.pop(0)()

            def unit(st_fn, dt_ap, col_view, scale, exp_bias, pv_fn, kq_bufs):
                b = ST_BANKS[nxt("st", 4)]
                st_fn(bank(b), PB[b], kq_bufs)
                pi_ = nxt("pt", 4)
                if dt_ap is None:
                    S.op("act", lambda e: e.activation(out=col_view(pT[pi_]), in_=col_view(bank(b)), func=AF.Exp, bias=exp_bias),
                         reads=[PB[b]], writes=[pT_bf[pi_]])
                else:
                    si = nxt("sb", 3)
                    S.op("dve", lambda e: e.scalar_tensor_tensor(
                        out=col_view(sbias[si]), in0=col_view(dt_ap), scalar=scale, in1=col_view(bank(b)), op0=ALU.mult, op1=ALU.add),
                        reads=[PB[b], const_bf], writes=[sbias_bf[si]])
                    S.op("act", lambda e: e.activation(out=col_view(pT[pi_]), in_=col_view(sbias[si]), func=AF.Exp, bias=exp_bias),
                         reads=[sbias_bf[si]], writes=[pT_bf[pi_]])
                pend.append(lambda: pv_fn(pT[pi_], pT_bf[pi_]))
                flush(LAG)
                filler_pop()

            def normalize(ob, ncols, hh, obuf, col0):
                ri = nxt("rl", 2)
                if NORM_MODE == 4:
                    h2 = ncols // 2
                    T, T2 = rlT[ri], rl2[ri]
                    S.op("act", lambda e: e.activation(out=T[0:64, 0:h2], in_=bank(ob)[64:128, 0:h2], func=AF.Copy),
                         reads=[PB[ob]], writes=[rl_bf[ri]])
                    S.op("act", lambda e: e.activation(out=T[64:128, 0:h2], in_=bank(ob)[64:128, h2:ncols], func=AF.Copy),
                         reads=[PB[ob]], writes=[rl_bf[ri]])
                    S.op("dve", lambda e: e.reciprocal(out=T[:, 0:h2], in_=T[:, 0:h2]), reads=[rl_bf[ri]], writes=[rl_bf[ri]])
                    S.op("act", lambda e: e.activation(out=T2[:, 0:h2], in_=T[64:128, 0:h2], func=AF.Copy),
                         reads=[rl_bf[ri]], writes=[rl_bf[ri]])
                    r_ = slice(64 * hh, 64 * hh + 64)
                    S.op("dve", lambda e: e.tensor_tensor(out=oT[obuf][r_, col0:col0 + h2], in0=bank(ob)[0:64, 0:h2],
                                                          in1=T[0:64, 0:h2], op=ALU.mult),
                         reads=[PB[ob], rl_bf[ri]], writes=[oT_bf[obuf]])
                    S.op("dve", lambda e: e.tensor_tensor(out=oT[obuf][r_, col0 + h2:col0 + ncols], in0=bank(ob)[0:64, h2:ncols],
                                                          in1=T2[:, 0:h2], op=ALU.mult),
                         reads=[PB[ob], rl_bf[ri]], writes=[oT_bf[obuf]])
                    return
                if NORM_MODE == 0:
                    S.op("dve", lambda e: e.reciprocal(out=rl[ri][:, 0:ncols], in_=bank(ob)[64:128, 0:ncols]),
                         reads=[PB[ob]], writes=[rl_bf[ri]])
                elif NORM_MODE == 3:
                    S.op("act", lambda e: e.activation(out=rl[ri][:, 0:ncols], in_=bank(ob)[64:128, 0:ncols], func=AF.Copy),
                         reads=[PB[ob]], writes=[rl_bf[ri]])
                    S.op("pool", lambda e: e.tensor_tensor(out=rl[ri][:, 0:ncols], in0=rl[ri][:, 0:ncols], in1=mones[:, 0:ncols], op=ALU.pow),
                         reads=[rl_bf[ri], const_bf], writes=[rl_bf[ri]])
                else:
                    S.op("act", lambda e: e.activation(out=rl[ri][:, 0:ncols], in_=bank(ob)[64:128, 0:ncols], func=AF.Ln),
                         reads=[PB[ob]], writes=[rl_bf[ri]])
                    S.op("act", lambda e: e.activation(out=rl[ri][:, 0:ncols], in_=rl[ri][:, 0:ncols], func=AF.Exp, scale=-1.0),
                         reads=[rl_bf[ri]], writes=[rl_bf[ri]])
                S.op("dve", lambda e: e.tensor_tensor(out=oT[obuf][64 * hh:64 * hh + 64, col0:col0 + ncols],
                                                      in0=bank(ob)[0:64, 0:ncols], in1=rl[ri][:, 0:ncols], op=ALU.mult),
                     reads=[PB[ob], rl_bf[ri]], writes=[oT_bf[obuf]])

            def mm(out_ap, lhsT, rhs, start, stop, reads, wbuf):
                S.op("pe", lambda e: e.matmul(out_ap, lhsT=lhsT, rhs=rhs, start=start, stop=stop, skip_group_check=True),
                     reads=reads, writes=[wbuf])

            def dilated_head(pi, hh, obuf):
                qb, kb, qb_bf, kb_bf = qk_sets[pi % 2]
                slope = SLOPES_A[2 * pi + hh]
                r0, r1 = 64 * hh, 64 * hh + 64
                D1 = dtab[:, 0:512]
                for j in range(4):
                    ob = 5 + nxt("o", 2)
                    state = {"first": True}

                    def pv(out_ap, lhsT, rhs, rd, last=False, state=state, ob=ob):
                        mm(out_ap, lhsT, rhs, state["first"], last, rd, PB[ob])
                        state["first"] = False

                    for bi in range(2):
                        n0 = 4 * j + 2 * bi
                        c0 = 0 if n0 >= 1 else 128

                        def st_fn(bk, pbuf, kq, n0=n0):
                            if n0 >= 1:
                                mm(bk[:, 0:128], kb[r0:r1, 128 * (n0 - 1):128 * n0], qb[r0:r1, 128 * n0:128 * (n0 + 1)], True, True, kq, pbuf)
                            mm(bk[:, 128:384], kb[r0:r1, 128 * n0:128 * (n0 + 1)], qb[r0:r1, 128 * n0:128 * (n0 + 2)], True, True, kq, pbuf)
                            mm(bk[:, 384:512], kb[r0:r1, 128 * (n0 + 1):128 * (n0 + 2)], qb[r0:r1, 128 * (n0 + 1):128 * (n0 + 2)], True, True, kq, pbuf)

                        def pv_fn(pt, ptb, n0=n0, j=j, pv=pv, ob=ob):
                            for qi in range(2):
                                n = n0 + qi
                                o_ap = bank(ob)[:, 128 * (n - 4 * j):128 * (n - 4 * j + 1)]
                                if n >= 1:
                                    pv(o_ap, vaug[0][:, n - 1, hh, :], pt[:, 256 * qi:256 * qi + 128], [vaug_bf[0], ptb])
                                pv(o_ap, vaug[0][:, n, hh, :], pt[:, 256 * qi + 128:256 * qi + 256], [vaug_bf[0], ptb])

                        unit(st_fn, D1, (lambda ap, c0=c0: ap[:, c0:512]), -slope, 0.0, pv_fn, [qb_bf, kb_bf])
                    for bi in range(2):
                        def st_fn(bk, pbuf, kq, bi=bi, j=j):
                            for u in range(2):
                                r4 = 2 * bi + u
                                q_ap = qb[r0:r1, 512 * j + r4:512 * (j + 1):4]
                                if j >= 1:
                                    mm(bk[:, 256 * u:256 * u + 128], kb[r0:r1, 512 * (j - 1) + r4:512 * j:4], q_ap, True, True, kq, pbuf)
                                mm(bk[:, 256 * u + 128:256 * u + 256], kb[r0:r1, 512 * j + r4:512 * (j + 1):4], q_ap, True, True, kq, pbuf)

                        def pv_fn(pt, ptb, bi=bi, j=j, pv=pv, ob=ob):
                            for u in range(2):
                                r4 = 2 * bi + u
                                o_ap = bank(ob)[:, r4:512:4]
                                if j >= 1:
                                    pv(o_ap, vaug[1][:, 4 * r4 + j - 1, hh, :], pt[:, 256 * u:256 * u + 128], [vaug_bf[1], ptb])
                                pv(o_ap, vaug[1][:, 4 * r4 + j, hh, :], pt[:, 256 * u + 128:256 * u + 256], [vaug_bf[1], ptb])

                        if j >= 1:
                            cv = lambda ap: ap
                        else:
                            cv = lambda ap: ap.rearrange("p (u c) -> p u c", c=256)[:, :, 128:256]
                        unit(st_fn, D1, cv, -slope * 4.0, 0.0, pv_fn, [qb_bf, kb_bf])
                    def st_fn(bk, pbuf, kq, j=j):
                        for r16 in range(16):
                            mm(bk[:, 32 * r16:32 * (r16 + 1)], kb[r0:r1, r16:SEQ:16],
                               qb[r0:r1, 512 * j + r16:512 * (j + 1):16], True, True, kq, pbuf)

                    def pv_fn(pt, ptb, j=j, pv=pv, ob=ob):
                        for r16 in range(16):
                            pv(bank(ob)[:, r16:512:16], vaug[2][:, r16, hh, :], pt[:, 32 * r16:32 * (r16 + 1)],
                               [vaug_bf[2], ptb], last=(r16 == 15))

                    unit(st_fn, dtab[:, 512 * (1 + j):512 * (2 + j)], (lambda ap: ap), -slope * 16.0, 0.0, pv_fn, [qb_bf, kb_bf])
                    pend.append(lambda ob=ob, j=j: normalize(ob, 512, hh, obuf, 512 * j))

            def moba_head(pi, hh, obuf):
                qb, kb, qb_bf, kb_bf = qk_sets[pi % 2]
                hB = 2 * (pi - 6) + hh
                slope = SLOPES_B[hB]
                proj_qk(pi, 0, qb, qb_bf, rows=(64 * hh, 64 * hh + 64))
                proj_qk(pi, 1, kb, kb_bf, rows=(64 * hh, 64 * hh + 64))
                S.dma("pool", kb[64:74, :], erows_d[12 * hB:12 * hB + 10, :], writes=[kb_bf])
                S.dma("pool", qb[72:74, :], erows_d[12 * hB + 10:12 * hB + 12, :], writes=[qb_bf])
                S.op("dve", lambda e: e.tensor_reduce(out=kmf, in_=kb[0:64, :].rearrange("p (m t) -> p m t", t=256),
                                                      axis=mybir.AxisListType.X, op=ALU.add), reads=[kb_bf], writes=[misc_bf])
                S.op("dve", lambda e: e.tensor_copy(out=kmh, in_=kmf), reads=[misc_bf], writes=[misc_bf])
                S.op("dve", lambda e: e.tensor_tensor(out=kmd, in0=kmf, in1=kmh, op=ALU.subtract), reads=[misc_bf], writes=[misc_bf])
                S.op("dve", lambda e: e.tensor_copy(out=kml, in_=kmd), reads=[misc_bf], writes=[misc_bf])
                for t in range(NT):
                    mm(bank(7)[:, 8 * t:8 * t + 8], qb[0:64, 128 * t:128 * (t + 1)], kmh, True, False, [qb_bf, misc_bf], PB[7])
                    mm(bank(7)[:, 8 * t:8 * t + 8], qb[0:64, 128 * t:128 * (t + 1)], kml, False, True, [qb_bf, misc_bf], PB[7])
                S.op("dve", lambda e: e.tensor_tensor(out=gsb, in0=bank(7)[:, 0:128].rearrange("p (a m) -> p a m", m=8),
                                                      in1=gcon[:, 0:128].rearrange("p (a m) -> p a m", m=8), op=ALU.add),
                     reads=[PB[7], const_bf], writes=[misc_bf])
                for t in range(NT):
                    S.op("dve", lambda e, t=t: e.max(out=top8[:, t, :], in_=gsb[:, t, :]), reads=[misc_bf], writes=[misc_bf])
                S.op("dve", lambda e: e.tensor_scalar(out=thr, in0=top8[:, :, 2], scalar1=-1.0e29, scalar2=None, op0=ALU.max),
                     reads=[misc_bf], writes=[misc_bf])
                S.op("dve", lambda e: e.tensor_tensor(out=ind, in0=gsb, in1=thr.unsqueeze(2).broadcast_to([128, 16, 8]), op=ALU.is_ge),
                     reads=[misc_bf], writes=[misc_bf])
                S.op("dve", lambda e: e.tensor_tensor(out=ind, in0=ind, in1=gcon[:, 128:256].rearrange("p (a m) -> p a m", m=8), op=ALU.max),
                     reads=[misc_bf, const_bf], writes=[misc_bf])
                S.op("dve", lambda e: e.tensor_scalar(out=bsel, in0=ind, scalar1=-1.0, scalar2=-NEGB, op0=ALU.add, op1=ALU.mult),
                     reads=[misc_bf], writes=[misc_bf])
                psb = bank(7).bitcast(BF16)
                for half in range(2):
                    for t8 in range(8):
                        t = 8 * half + t8
                        S.op("pe", lambda e, t=t, t8=t8: e.transpose(out=psb[0:8, 128 * t8:128 * (t8 + 1)], in_=bsel[:, t, :], identity=ident[:]),
                             reads=[misc_bf, const_bf], writes=[PB[7]])
                    S.op("act", lambda e, half=half: e.activation(out=qb[64:72, 1024 * half:1024 * (half + 1)], in_=psb[0:8, :], func=AF.Copy),
                         reads=[PB[7]], writes=[qb_bf])
                DMP = dtab[:, 2560:3072]
                DMO = dtab[:, 3072:3584]
                for n in range(8):
                    ob = 5 + nxt("o", 2)
                    state = {"first": True}

                    def pv(out_ap, lhsT, rhs, rd, last=False, state=state, ob=ob):
                        mm(out_ap, lhsT, rhs, state["first"], last, rd, PB[ob])
                        state["first"] = False

                    for m in range(n + 1):
                        def st_fn(bk, pbuf, kq, n=n, m=m):
                            for h in range(2):
                                mm(bk[:, 256 * h:256 * (h + 1)], kb[0:74, 256 * m + 128 * h:256 * m + 128 * (h + 1)],
                                   qb[0:74, 256 * n:256 * (n + 1)], True, True, kq, pbuf)

                        def pv_fn(pt, ptb, n=n, m=m, pv=pv, ob=ob):
                            for h in range(2):
                                pv(bank(ob)[:, 0:256], vaug[0][:, 2 * m + h, hh, :], pt[:, 256 * h:256 * (h + 1)],
                                   [vaug_bf[0], ptb], last=(m == n and h == 1))

                        unit(st_fn, DMO if m == n else None, (lambda ap: ap), -1.0, -slope * 256.0 * (n - m), pv_fn, [qb_bf, kb_bf])
                    pend.append(lambda ob=ob, n=n: normalize(ob, 256, hh, obuf, 256 * n))

            def out_proj(pi, obuf):
                for t in range(NT):
                    yb = (2, 5)[nxt("ybo", 2)]
                    for h in range(2):
                        S.op("pe", lambda e, yb=yb, h=h, t=t: e.matmul(
                            bank(yb + h), lhsT=oT[obuf][:, 128 * t:128 * (t + 1)], rhs=wout_sl[pi % 2][:, 512 * h:512 * (h + 1)],
                            start=True, stop=True), reads=[oT_bf[obuf], wout_bf[pi % 2]], writes=[PB[yb + h]])
                    S.op("dve", lambda e, yb=yb, t=t: e.scalar_tensor_tensor(
                        out=R[:, t, :], in0=bank(yb, 2), scalar=C_MIX, in1=R[:, t, :], op0=ALU.mult, op1=ALU.add),
                        reads=[PB[yb], PB[yb + 1], R_bf[t]], writes=[R_bf[t]])

            def attend(pi):
                obuf = pi % 2
                if pi < 6:
                    for hh in range(2):
                        dilated_head(pi, hh, obuf)
                else:
                    for hh in range(2):
                        moba_head(pi, hh, obuf)
                flush(0)
                filler_flush()

            def project(pi, qk_now=True):
                qb_, kb_, qbf_, kbf_ = qk_sets[pi % 2]
                if pi == 0:
                    load_w(pi)
                    proj_qk(pi, 0, qb_, qbf_, chunks=(0, 1, 2), pops=3)
                    proj_qk(pi, 1, kb_, kbf_, chunks=(0, 1, 2), pops=3)
                    bg_flush()
                    proj_qk(pi, 0, qb_, qbf_, chunks=(3,))
                    proj_qk(pi, 1, kb_, kbf_, chunks=(3,))
                    proj_v(pi, (0, 1, 2))
                elif pi < 6:
                    proj_v(pi, (0, 1, 2))
                else:
                    proj_v(pi, (0,))

            def queue_qk(pi):
                load_w(pi)
                if pi < 6:
                    qb_, kb_, qbf_, kbf_ = qk_sets[pi % 2]
                    proj_qk(pi, 0, qb_, qbf_, defer=fillers)
                    proj_qk(pi, 1, kb_, kbf_, defer=fillers)

            project(0)
            for pi in range(8):
                if pi + 1 < 8:
                    queue_qk(pi + 1)
                attend(pi)
                out_proj(pi, pi % 2)
                if pi + 1 < 8:
                    project(pi + 1)
            for g in range(4):
                bg.extend(ln_tasks([4 * g + tt for tt in range(4)], post_ln2))
                if g == 0:
                    bg_flush()

        def post_ln2(t):
            def a():
                if dbg:
                    S.dma("pool", dbg_h2[128 * t:128 * (t + 1), :], R[:, t, :], reads=[R_bf[t]], writes=[out_bf])
            return [a]

        cast_ffn(0)
        for s in range(n_seq):
            ffn_phase(s, 0, 0, True)
            if s == 0:
                cast_attn()
                cast_ffn(1)
            attn_phase(s)
            ffn_phase(s, 1, 2, False)
        bg_flush()
        S.emit(block)
    return nc


_PROGRAM_CACHE = {}


def _layout_weights(inp):
    def gu(gate, up):
        def r(w):
            return w.reshape(KC, 128, FC, 128).transpose(2, 1, 0, 3)
        a = np.stack([r(gate), r(up)], axis=2)
        return np.ascontiguousarray(a.reshape(FC * 128, 2048))

    def dn(w):
        return np.ascontiguousarray(w.reshape(FC, 128, DM).transpose(1, 0, 2).reshape(128, FC * DM))

    w_in = inp["w_in"][0]
    blocks = []
    w3 = w_in.reshape(KC, 128, 3072)
    for pi in range(8):
        if pi < 6:
            cols = (128 * pi, 768 + 128 * pi, 1536 + 128 * pi)
        else:
            cols = (2304 + 128 * (pi - 6), 2560 + 128 * (pi - 6), 2816 + 128 * (pi - 6))
        per_t = [w3[:, :, c:c + 128].transpose(1, 0, 2) for c in cols]
        blocks.append(np.stack(per_t, axis=1).reshape(128, 3072))
    win = np.ascontiguousarray(np.concatenate(blocks, axis=0))
    ln = np.ascontiguousarray(np.stack([inp["ln1_g"][0], inp["ln1_b"][0], inp["ln2_g"][0], inp["ln2_b"][0],
                                        inp["ln3_g"][0], inp["ln3_b"][0]], axis=0))
    return {
        "wgu1": gu(inp["ffn1_gate"][0], inp["ffn1_up"][0]),
        "wgu2": gu(inp["ffn2_gate"][0], inp["ffn2_up"][0]),
        "wd1": dn(inp["ffn1_down"][0]),
        "wd2": dn(inp["ffn2_down"][0]),
        "win": win,
        "wout": np.ascontiguousarray(inp["w_out"][0]),
        "ln": ln,
    }


def kernel(**inputs):
    inp = {k: np.asarray(v, dtype=np.float32) for k, v in inputs.items()}
    x = inp["x"]
    assert x.shape == (BATCH, SEQ, DM)
    shared = _layout_weights(inp)
    dtab, gcon, erows, ident = _const_tables()
    shared.update({"dtab": dtab, "gcon": gcon, "erows": erows, "ident": ident})
    if "nc" not in _PROGRAM_CACHE:
        _PROGRAM_CACHE["nc"] = build_program(BPC)
    nc = _PROGRAM_CACHE["nc"]
    in_maps = []
    for c in range(NCORES):
        m = dict(shared)
        m["x"] = np.ascontiguousarray(x[c * BPC:(c + 1) * BPC].reshape(BPC * SEQ, DM))
        in_maps.append(m)
    res = run_bass_kernel_spmd(nc, in_maps, core_ids=list(range(NCORES)))
    outs = [np.asarray(r["out"]).reshape(BPC, SEQ, DM) for r in res.results]
    return np.concatenate(outs, axis=0).astype(np.float32)
```
